# Optimizing a Trainium2 kernel written in Bass

```python
import math
import jax, jax.numpy as jnp
from jax import lax
import numpy as np

D_MODEL = 1024
BATCH = 8
SEQ = 4096
DEPTH = 2

N_BRANCH = 4
BRANCH_W = D_MODEL // 4
POOL_WINDOWS = (2, 4, 8, 16)
POOL_GW = BRANCH_W // len(POOL_WINDOWS)
CONV_W = 3
DA_HEADS = 4
DA_HEAD_DIM = BRANCH_W // (2 * DA_HEADS)
DA_V_DIM = 2 * DA_HEAD_DIM
ROPE_THETA = 10000.0
Q_BLOCK = 128
SG_CHUNK = 128
SG_GROUPS = 4
SG_GW = BRANCH_W // SG_GROUPS
N_GROUPS = 4
EXPERTS_PER_GROUP = 4
N_EXPERTS = N_GROUPS * EXPERTS_PER_GROUP
TOP_K = 2
EXPERT_HIDDEN = 256
IN_COLS = BRANCH_W + 3 * BRANCH_W + 3 * BRANCH_W + 2 * BRANCH_W + N_BRANCH * D_MODEL
LN_EPS = 1e-5
NEG_INF = -1e30
DEEPNORM_ALPHA = (2 * DEPTH) ** 0.25
DEEPNORM_BETA = (8 * DEPTH) ** -0.25

kernel_name = 'hybrid_gated_pool_conv_diffattn_sgmlp_hmoe'


def layer_norm(x, g, b):
    xf = x.astype(jnp.float32)
    mu = jnp.mean(xf, axis=-1, keepdims=True)
    var = jnp.mean(jnp.square(xf - mu), axis=-1, keepdims=True)
    return ((xf - mu) * lax.rsqrt(var + LN_EPS)).astype(x.dtype) * g + b


def rotary(x, cos, sin):
    half = x.shape[-1] // 2
    c = cos[None, :, None, None, :]
    s = sin[None, :, None, None, :]
    xf = x.astype(jnp.float32)
    x1, x2 = xf[..., :half], xf[..., half:]
    return jnp.concatenate([x1 * c - x2 * s, x2 * c + x1 * s], axis=-1).astype(x.dtype)


def causal_multiscale_pool(a, pool_w, pool_scale):
    S = a.shape[1]
    af = a.astype(jnp.float32)
    csum = jnp.pad(jnp.cumsum(af, axis=1), ((0, 0), (1, 0), (0, 0)))
    t = jnp.arange(S)
    pooled = []
    for g, w in enumerate(POOL_WINDOWS):
        cg = csum[..., g * POOL_GW:(g + 1) * POOL_GW]
        lo = jnp.maximum(t + 1 - w, 0)
        win_sum = cg[:, 1:] - jnp.take(cg, lo, axis=1)
        count = jnp.minimum(t + 1, w).astype(jnp.float32)
        pooled.append(win_sum / count[None, :, None])
    pooled = jnp.stack(pooled, axis=2)
    d = (pooled - af.reshape(pooled.shape)).astype(a.dtype)
    y = jnp.einsum('bsgc,gcd->bsgd', d, pool_w)
    return y.reshape(a.shape) * pool_scale


def short_gated_conv(gb, gc, h, conv_w):
    S = h.shape[1]
    z = jnp.pad(gc * h, ((0, 0), (CONV_W - 1, 0), (0, 0)))
    y = z[:, 0:S] * conv_w[0]
    for j in range(1, CONV_W):
        y = y + z[:, j:j + S] * conv_w[j]
    return gb * y


def differential_attention(q, k, v, cos, sin, lam, lam_init, subln_g):
    q = rotary(q, cos, sin)
    k = rotary(k, cos, sin)
    S = q.shape[1]
    scale = DA_HEAD_DIM ** -0.5
    outs = []
    for qb in range(S // Q_BLOCK):
        q0 = qb * Q_BLOCK
        kv = q0 + Q_BLOCK
        s = jnp.einsum('bqhmd,bkhmd->bhmqk', q[:, q0:kv], k[:, :kv]).astype(jnp.float32) * scale
        causal = (q0 + jnp.arange(Q_BLOCK))[:, None] >= jnp.arange(kv)[None, :]
        p = jax.nn.softmax(jnp.where(causal, s, NEG_INF), axis=-1)
        a = p[:, :, 0] - lam * p[:, :, 1]
        outs.append(jnp.einsum('bhqk,bkhe->bqhe', a.astype(v.dtype), v[:, :kv]))
    o = jnp.concatenate(outs, axis=1).astype(jnp.float32)
    o = o * lax.rsqrt(jnp.mean(o * o, axis=-1, keepdims=True) + LN_EPS)
    o = (o * (1.0 - lam_init)).astype(v.dtype) * subln_g
    return o.reshape(o.shape[0], S, DA_HEADS * DA_V_DIM)


def chunked_spatial_gating(u, v, ln_g, ln_b, w_s, b_s):
    B, S, _ = v.shape
    v = layer_norm(v, ln_g, ln_b)
    vc = v.reshape(B, S // SG_CHUNK, SG_CHUNK, SG_GROUPS, SG_GW)
    mask = jnp.tril(jnp.ones((SG_CHUNK, SG_CHUNK), dtype=bool))
    ws = jnp.where(mask[None], w_s, jnp.zeros_like(w_s))
    y = jnp.einsum('gts,bnsgc->bntgc', ws, vc) + b_s.T[None, None, :, :, None]
    return u * y.reshape(B, S, BRANCH_W)


def token_mixer(x, cos, sin, lam_init, w_in, pool_w, pool_scale, conv_w, lam_q1, lam_k1, lam_q2, lam_k2,
                subln_g, sg_ln_g, sg_ln_b, sg_w, sg_b, w_branch, w_o):
    B, S, _ = x.shape
    W = BRANCH_W
    proj = x @ w_in
    p_pool, p_conv, p_attn, p_sg, p_gate = jnp.split(proj, [W, 4 * W, 7 * W, 9 * W], axis=-1)
    y_a = causal_multiscale_pool(p_pool, pool_w, pool_scale)
    gb, gc, h = jnp.split(p_conv, 3, axis=-1)
    y_b = short_gated_conv(gb, gc, h, conv_w)
    q, k, v = jnp.split(p_attn, 3, axis=-1)
    q = q.reshape(B, S, DA_HEADS, 2, DA_HEAD_DIM)
    k = k.reshape(B, S, DA_HEADS, 2, DA_HEAD_DIM)
    v = v.reshape(B, S, DA_HEADS, DA_V_DIM)
    lam = (jnp.exp(jnp.sum(lam_q1.astype(jnp.float32) * lam_k1.astype(jnp.float32)))
           - jnp.exp(jnp.sum(lam_q2.astype(jnp.float32) * lam_k2.astype(jnp.float32))) + lam_init)
    y_c = differential_attention(q, k, v, cos, sin, lam, lam_init, subln_g)
    uv = jax.nn.gelu(p_sg)
    u, vv = jnp.split(uv, 2, axis=-1)
    y_d = chunked_spatial_gating(u, vv, sg_ln_g, sg_ln_b, sg_w, sg_b)
    gates = jax.nn.sigmoid(p_gate.reshape(B, S, N_BRANCH, D_MODEL))
    branches = (y_a, y_b, y_c, y_d)
    merged = gates[:, :, 0] * (branches[0] @ w_branch[0])
    for i in range(1, N_BRANCH):
        merged = merged + gates[:, :, i] * (branches[i] @ w_branch[i])
    return merged @ w_o


def hierarchical_moe(x, w_rg, b_rg, w_re, b_re, w_gate, w_up, w_down):
    gl = (x @ w_rg + b_rg).astype(jnp.float32)
    pg = jax.nn.softmax(gl, axis=-1)
    g_sel = jnp.argmax(gl, axis=-1)
    p_sel = jnp.take_along_axis(pg, g_sel[..., None], axis=-1)
    el = (jnp.einsum('bsd,gde->bsge', x, w_re) + b_re).astype(jnp.float32)
    el_sel = jnp.take_along_axis(el, g_sel[..., None, None], axis=2)[:, :, 0]
    top_v, top_i = lax.top_k(el_sel, TOP_K)
    top_w = jax.nn.softmax(top_v, axis=-1) * p_sel
    expert_id = g_sel[..., None] * EXPERTS_PER_GROUP + top_i
    gate = jnp.sum(jax.nn.one_hot(expert_id, N_EXPERTS, dtype=jnp.float32) * top_w[..., None], axis=-2)
    gate = gate.astype(x.dtype)
    hg = jnp.einsum('bsd,edh->bseh', x, w_gate)
    hu = jnp.einsum('bsd,edh->bseh', x, w_up)
    act = jax.nn.silu(hg) * hu * gate[..., None]
    return jnp.einsum('bseh,ehd->bsd', act, w_down)


def setup_inputs(seed: int = 0) -> dict:
    key = jax.random.key(seed)
    ks = jax.random.split(key, 32)
    f32 = jnp.float32
    L = DEPTH

    def nrm(k, shape, scale):
        return jax.random.normal(k, shape, f32) * scale

    return {
        'x': nrm(ks[0], (BATCH, SEQ, D_MODEL), 1.0),
        'w_in': nrm(ks[1], (L, D_MODEL, IN_COLS), D_MODEL ** -0.5),
        'pool_w': nrm(ks[2], (L, len(POOL_WINDOWS), POOL_GW, POOL_GW), POOL_GW ** -0.5),
        'pool_scale': 1.0 + nrm(ks[3], (L, BRANCH_W), 0.02),
        'conv_w': nrm(ks[4], (L, CONV_W, BRANCH_W), CONV_W ** -0.5),
        'lam_q1': nrm(ks[5], (L, DA_HEAD_DIM), 0.1),
        'lam_k1': nrm(ks[6], (L, DA_HEAD_DIM), 0.1),
        'lam_q2': nrm(ks[7], (L, DA_HEAD_DIM), 0.1),
        'lam_k2': nrm(ks[8], (L, DA_HEAD_DIM), 0.1),
        'subln_g': 1.0 + nrm(ks[9], (L, DA_V_DIM), 0.02),
        'sg_ln_g': 1.0 + nrm(ks[10], (L, BRANCH_W), 0.02),
        'sg_ln_b': nrm(ks[11], (L, BRANCH_W), 0.02),
        'sg_w': nrm(ks[12], (L, SG_GROUPS, SG_CHUNK, SG_CHUNK), SG_CHUNK ** -0.5),
        'sg_b': 1.0 + nrm(ks[13], (L, SG_GROUPS, SG_CHUNK), 0.02),
        'w_branch': nrm(ks[14], (L, N_BRANCH, BRANCH_W, D_MODEL), BRANCH_W ** -0.5),
        'w_o': nrm(ks[15], (L, D_MODEL, D_MODEL), D_MODEL ** -0.5 * DEEPNORM_BETA),
        'ln1_g': 1.0 + nrm(ks[16], (L, D_MODEL), 0.02),
        'ln1_b': nrm(ks[17], (L, D_MODEL), 0.02),
        'w_rg': nrm(ks[18], (L, D_MODEL, N_GROUPS), D_MODEL ** -0.5),
        'b_rg': nrm(ks[19], (L, N_GROUPS), 0.01),
        'w_re': nrm(ks[20], (L, N_GROUPS, D_MODEL, EXPERTS_PER_GROUP), D_MODEL ** -0.5),
        'b_re': nrm(ks[21], (L, N_GROUPS, EXPERTS_PER_GROUP), 0.01),
        'w_gate': nrm(ks[22], (L, N_EXPERTS, D_MODEL, EXPERT_HIDDEN), D_MODEL ** -0.5),
        'w_up': nrm(ks[23], (L, N_EXPERTS, D_MODEL, EXPERT_HIDDEN), D_MODEL ** -0.5),
        'w_down': nrm(ks[24], (L, N_EXPERTS, EXPERT_HIDDEN, D_MODEL), EXPERT_HIDDEN ** -0.5 * DEEPNORM_BETA),
        'ln2_g': 1.0 + nrm(ks[25], (L, D_MODEL), 0.02),
        'ln2_b': nrm(ks[26], (L, D_MODEL), 0.02),
    }


def reference(x, w_in, pool_w, pool_scale, conv_w, lam_q1, lam_k1, lam_q2, lam_k2, subln_g, sg_ln_g, sg_ln_b,
              sg_w, sg_b, w_branch, w_o, ln1_g, ln1_b, w_rg, b_rg, w_re, b_re, w_gate, w_up, w_down, ln2_g, ln2_b):
    S = x.shape[1]
    half = DA_HEAD_DIM // 2
    inv_freq = ROPE_THETA ** (-jnp.arange(half, dtype=jnp.float32) / half)
    ang = jnp.arange(S, dtype=jnp.float32)[:, None] * inv_freq[None, :]
    cos, sin = jnp.cos(ang), jnp.sin(ang)
    for l in range(DEPTH):
        lam_init = 0.8 - 0.6 * math.exp(-0.3 * l)
        mix = token_mixer(x, cos, sin, lam_init, w_in[l], pool_w[l], pool_scale[l], conv_w[l],
                          lam_q1[l], lam_k1[l], lam_q2[l], lam_k2[l], subln_g[l], sg_ln_g[l], sg_ln_b[l],
                          sg_w[l], sg_b[l], w_branch[l], w_o[l])
        x = layer_norm(DEEPNORM_ALPHA * x + mix, ln1_g[l], ln1_b[l])
        ffn = hierarchical_moe(x, w_rg[l], b_rg[l], w_re[l], b_re[l], w_gate[l], w_up[l], w_down[l])
        x = layer_norm(DEEPNORM_ALPHA * x + ffn, ln2_g[l], ln2_b[l])
    return x
```

```python
import math
import os
import numpy as np
from contextlib import ExitStack
import concourse.bass as bass
import concourse.mybir as mybir
from concourse.bass_utils import run_bass_kernel_spmd

F32 = mybir.dt.float32
BF16 = mybir.dt.bfloat16
AF = mybir.ActivationFunctionType
ALU = mybir.AluOpType
AX = mybir.AxisListType

S = 4096
D = 1024
T = 512
NTILES = S // T
DEPTH = 2
IN_COLS = 6400
LN_EPS = 1e-5
ALPHA = (2 * DEPTH) ** 0.25
SCALE = 32 ** -0.5
ENGS = ("pe", "act", "dve", "pool", "sp")


class Buf:
    __slots__ = ("name", "lw", "rd", "excl", "lws")

    def __init__(self, name, excl=False):
        self.name = name
        self.lw = None
        self.lws = False
        self.rd = {}
        self.excl = excl


class Prog:
    def __init__(self, nc, same_engine_sync=False):
        self.nc = nc
        self.same = same_engine_sync
        self.streams = {e: [] for e in ENGS}
        self.count = {}
        self.seen = {e: {} for e in ENGS}
        self.semkeys = []
        for e in ENGS:
            self._newsem(("eng", e))
        self.n_ops = 0

    def _newsem(self, key):
        if key not in self.count:
            self.count[key] = 0
            self.semkeys.append(key)

    def _deps(self, eng, reads, writes):
        deps = {}
        me = ("eng", eng)
        same_needed = self.same == 1
        for r in reads:
            if r.lw is not None:
                k, c = r.lw
                if k == me and r.lws:
                    same_needed = True
                if deps.get(k, 0) < c:
                    deps[k] = c
        for w in writes:
            if w.lw is not None:
                k, c = w.lw
                if k == me and w.lws:
                    same_needed = True
                if deps.get(k, 0) < c:
                    deps[k] = c
            for k, c in w.rd.items():
                if deps.get(k, 0) < c:
                    deps[k] = c
        out = []
        seen = self.seen[eng]
        for k, c in deps.items():
            if k == me and (eng == "pe" or not same_needed):
                continue
            if seen.get(k, 0) >= c:
                continue
            seen[k] = c
            out.append((k, c))
        return out

    def op(self, eng, fn, reads=(), writes=(), small=False):
        ex = [r for r in reads if r.excl and r not in writes]
        if ex:
            writes = list(writes) + ex
        for k, c in self._deps(eng, reads, writes):
            self.streams[eng].append(("wait", k, c))
        key = ("eng", eng)
        self.count[key] += 1
        c = self.count[key]
        self.streams[eng].append(("op", fn, key, 1))
        for r in reads:
            r.rd[key] = c
        for w in writes:
            w.lw = (key, c)
            w.lws = small
            w.rd = {}
        self.n_ops += 1

    def dma(self, eng, fn, slot, reads=(), writes=()):
        key = ("dma", slot)
        self._newsem(key)
        for k, c in self._deps(eng, reads, writes):
            self.streams[eng].append(("wait", k, c))
        self.count[key] += 16
        c = self.count[key]
        self.streams[eng].append(("op", fn, key, 16))
        for r in reads:
            r.rd[key] = c
        for w in writes:
            w.lw = (key, c)
            w.lws = False
            w.rd = {}
        self.n_ops += 1

    def wait_all(self, eng, bufs):
        for k, c in self._deps(eng, bufs, bufs):
            self.streams[eng].append(("wait", k, c))

    def barrier(self, engs=("pe", "act", "dve")):
        for e in engs:
            for o in engs:
                if o == e:
                    continue
                k = ("eng", o)
                c = self.count[k]
                if c > 0 and self.seen[e].get(k, 0) < c:
                    self.seen[e][k] = c
                    self.streams[e].append(("wait", k, c))

    def emit(self, stack):
        nc = self.nc
        sems = {}
        for k in self.semkeys:
            sems[k] = stack.enter_context(nc.semaphore(("s_%s_%s" % (k[0], str(k[1]))).replace(" ", "").replace(",", "_").replace("(", "").replace(")", "").replace("'", "")))
        block = stack.enter_context(nc.Block())
        streams = self.streams

        def run(engobj, name):
            for item in streams[name]:
                if item[0] == "wait":
                    engobj.wait_ge(sems[item[1]], item[2])
                else:
                    _, fn, key, inc = item
                    fn(engobj).then_inc(sems[key], inc)

        @block.tensor
        def _(e):
            run(e, "pe")

        @block.scalar
        def _(e):
            run(e, "act")

        @block.vector
        def _(e):
            run(e, "dve")

        @block.gpsimd
        def _(e):
            run(e, "pool")

        @block.sync
        def _(e):
            run(e, "sp")


def host_consts():
    c = {}
    c["c_ident"] = np.eye(128, dtype=np.float32)
    k = np.arange(128)[:, None]
    q = np.arange(128)[None, :]
    c["c_maskneg"] = np.where(q >= k, 0.0, -30000.0).astype(np.float32)
    c["c_tril"] = (k <= q).astype(np.float32)
    perm = np.zeros((96, 96), np.float32)
    for po in range(96):
        d = po % 32
        pi = po + 16 if d < 16 else po - 16
        perm[pi, po] = 1.0
    c["c_perm"] = perm
    c["c_sel"] = np.eye(16, dtype=np.float32)
    half = 16
    inv_freq = (10000.0 ** (-np.arange(half, dtype=np.float32) / half)).astype(np.float32)
    ang = np.arange(S, dtype=np.float32)[:, None] * inv_freq[None, :]
    cos = np.cos(ang).astype(np.float32)
    sin = np.sin(ang).astype(np.float32)
    cosT = np.zeros((96, S), np.float32)
    sinT = np.zeros((96, S), np.float32)
    for p in range(96):
        d = p % 32
        j = d % 16
        cosT[p] = cos[:, j]
        sinT[p] = -sin[:, j] if d < 16 else sin[:, j]
    c["c_cos"] = cosT
    c["c_sin"] = sinT
    rc = np.zeros((2, 128, 2, T), np.float32)
    wins = {(0, 0): 2, (0, 1): 4, (1, 0): 8, (1, 1): 16}
    tt = np.arange(T)
    for ch in range(2):
        for hh in range(2):
            w = wins[(ch, hh)]
            rc[0, hh * 64:(hh + 1) * 64, ch, :] = 1.0 / np.minimum(tt + 1, w)
            rc[1, hh * 64:(hh + 1) * 64, ch, :] = 1.0 / w
    c["c_rc"] = rc
    return c


WEIGHT_SPECS = [
    ("w_in", [DEPTH, D, IN_COLS]), ("pool_w", [DEPTH, 4, 64, 64]), ("pool_scale", [DEPTH, 256]),
    ("conv_w", [DEPTH, 3, 256]), ("lam_q1", [DEPTH, 32]), ("lam_k1", [DEPTH, 32]), ("lam_q2", [DEPTH, 32]),
    ("lam_k2", [DEPTH, 32]), ("subln_g", [DEPTH, 64]), ("sg_ln_g", [DEPTH, 256]), ("sg_ln_b", [DEPTH, 256]),
    ("sg_w", [DEPTH, 4, 128, 128]), ("sg_b", [DEPTH, 4, 128]), ("w_branch", [DEPTH, 4, 256, D]),
    ("w_o", [DEPTH, D, D]), ("ln1_g", [DEPTH, D]), ("ln1_b", [DEPTH, D]), ("w_rg", [DEPTH, D, 4]),
    ("b_rg", [DEPTH, 4]), ("w_re", [DEPTH, 4, D, 4]), ("b_re", [DEPTH, 4, 4]), ("w_gate", [DEPTH, 16, D, 256]),
    ("w_up", [DEPTH, 16, D, 256]), ("w_down", [DEPTH, 16, 256, D]), ("ln2_g", [DEPTH, D]), ("ln2_b", [DEPTH, D]),
]
CONST_SPECS = [("c_ident", [128, 128]), ("c_maskneg", [128, 128]), ("c_tril", [128, 128]), ("c_perm", [96, 96]),
               ("c_sel", [16, 16]), ("c_cos", [96, S]), ("c_sin", [96, S]), ("c_rc", [2, 128, 2, T])]


def build(NT=NTILES, NL=DEPTH, dbg=()):
    nc = bass.Bass("TRN2", target_bir_lowering=False)
    dr = {}
    dr["x"] = nc.dram_tensor("x", [S, D], F32, kind="ExternalInput").ap()
    for name, shape in WEIGHT_SPECS + CONST_SPECS:
        dr[name] = nc.dram_tensor(name, shape, F32, kind="ExternalInput").ap()
    out_d = nc.dram_tensor("out", [S, D], F32, kind="ExternalOutput").ap()
    xs_d = nc.dram_tensor("xscratch", [S, D], F32).ap()
    NBLK = 2 * 90
    wscr = nc.dram_tensor("wscratch", [NBLK, 128, 4096], BF16).ap()
    dbg_d = {}
    for name, shape in dbg:
        dbg_d[name] = nc.dram_tensor("dbg_" + name, shape, F32, kind="ExternalOutput").ap()

    st = ExitStack()
    with st:
        def sb(name, shape, dt):
            return st.enter_context(nc.sbuf_tensor(name, shape, dt))

        def ps(name, shape, dt):
            return st.enter_context(nc.psum_tensor(name, shape, dt))

        P = Prog(nc, same_engine_sync=int(os.environ.get("K_SAME", "2")))

        PB = [ps("pb%d" % i, [128, 512], F32) for i in range(8)]
        BK = [Buf("bank%d" % i, excl=True) for i in range(8)]

        def pbf(i):
            return PB[i][:].bitcast(BF16)

        X = sb("X", [128, 4, D], F32)
        XT = sb("XT", [128, 8, T], BF16)
        XN = sb("XN", [128, 4, D], BF16)
        KC = sb("KC", [96, 3, S], BF16)
        QRZ = sb("QRZ", [96, 8, T], BF16)
        VC = sb("VC", [128, 32, 4, 65], BF16)
        NW = int(os.environ.get('K_NW', '3'))
        WS = [sb("WS%d" % i, [128, 8, 512], BF16) for i in range(NW)]
        NWD = int(os.environ.get('K_NWD', '4'))
        WD = [sb("WD%d" % i, [128, 2, 512], BF16) for i in range(NWD)]
        R = sb("R", [128, 18432], BF16)
        NTMP = 6
        TMP = [sb("TMP%d" % i, [128, 528], F32) for i in range(NTMP)]
        NPT = 3
        PT = [sb("PT%d" % i, [128, 512], BF16) for i in range(NPT)]
        LNG = sb("LNG", [128, D], F32)
        LNB = sb("LNB", [128, D], F32)
        A_ = [sb("Apool%d" % c, [128, 528], F32) for c in range(2)]
        Z_ = [sb("Zconv%d" % c, [128, 514], F32) for c in range(2)]
        PW = sb("PW", [128, 2, 128], BF16)
        PSC = sb("PSC", [128, 2], F32)
        CW = sb("CW", [128, 2, 3], F32)
        LAMV = sb("LAMV", [128, 4, 32], F32)
        LAMT = sb("LAMT", [128, 8], F32)
        SUBG = sb("SUBG", [128, 64], F32)
        SGG = sb("SGG", [128, 256], F32)
        SGBb = sb("SGBb", [128, 256], F32)
        SGWn = sb("SGWn", [128, 4, 128], BF16)
        SGW = sb("SGW", [128, 4, 128], BF16)
        SGB = sb("SGB", [128, 4], F32)
        WR = sb("WR", [128, 8, 20], BF16)
        BR = sb("BR", [128, 20], F32)
        IDf = sb("IDf", [128, 128], F32)
        ID = sb("ID", [128, 128], BF16)
        MNf = sb("MNf", [128, 128], F32)
        MN = sb("MN", [128, 128], BF16)
        TRIL = sb("TRIL", [128, 128], F32)
        PERMf = sb("PERMf", [96, 96], F32)
        PERM = sb("PERM", [96, 96], BF16)
        SELf = sb("SELf", [16, 16], F32)
        SEL = sb("SEL", [128, 16, 128], BF16)
        GTT = sb("GTT", [128, 512], BF16)
        ZER = sb("ZER", [128, 512], BF16)
        COS = sb("COS", [96, T], F32)
        SIN = sb("SIN", [96, T], F32)
        RC = sb("RC", [128, 2, T], F32)
        SM = sb("SM", [128, 864], F32)
        DTP = sb("DTP", [128, 2, 512], BF16)

        MACC = R[:, 0:8192].bitcast(F32).rearrange("p (j t) -> p j t", j=8)
        MT = R[:, 8192:12288].rearrange("p (j t) -> p j t", j=8)
        YT = [R[:, 12288 + i * 1024:12288 + (i + 1) * 1024].rearrange("p (c t) -> p c t", c=2) for i in range(4)]
        YC = R[:, 16384:17408].rearrange("p (s c) -> p s c", s=4)
        YD = R[:, 17408:18432].rearrange("p (s c) -> p s c", s=4)

        ACTT = R[:, 0:16384].rearrange("p (c t) -> p c t", c=32)

        B = {}
        for n in ["X", "XT", "VC", "MACC", "MT", "YC", "YD", "QR", "LNG", "LNB", "A0", "A1", "Z0", "Z1",
                  "PW", "PWf", "PSC", "CW", "LAMV", "LAMT", "SUBG", "SGG", "SGBb", "SGWn", "SGW", "SGB", "WR", "BR",
                  "IDf", "ID", "MNf", "MN", "TRIL", "PERMf", "PERM", "SELf", "SEL", "ZER", "COS", "SIN", "RC", "SM",
                  "out", "ACTT", "GTT", "QRZ", "XN", "DTP0", "DTP1"]:
            B[n] = Buf(n)
        for i in range(4):
            B["YT%d" % i] = Buf("YT%d" % i)
        BKC = [Buf("KC%d" % t) for t in range(NTILES)]
        BWS = [Buf("WS%d" % i) for i in range(NW)]
        BWD = [Buf("WD%d" % i) for i in range(NWD)]
        BTMP = [Buf("TMP%d" % i) for i in range(NTMP)]
        BPT = [Buf("PT%d" % i) for i in range(NPT)]
        BXS = [Buf("XS%d" % t) for t in range(NTILES)]
        RB = [Buf("RB%d" % i) for i in range(39)]
        BACT = [RB[i] for i in range(32)]

        def bMACC(j):
            return [RB[2 * j], RB[2 * j + 1]]

        def bMT(j):
            return [RB[16 + j]]

        def bYT(i):
            return [RB[24 + 2 * i], RB[25 + 2 * i]]

        bMTall = RB[16:24]
        bYC = [RB[32], RB[33]]
        bYD = [RB[34], RB[35]]
        BDBG = {}

        ctr = {"tmp": 0, "ws": 0, "wd": 0, "pt": 0, "ev": 0, "dbg": 0}

        def tmp():
            i = ctr["tmp"] % NTMP
            ctr["tmp"] += 1
            return TMP[i], BTMP[i]

        def wslot():
            i = ctr["ws"] % NW
            ctr["ws"] += 1
            return i

        def wdslot():
            i = ctr["wd"] % NWD
            ctr["wd"] += 1
            return i

        def ptslot():
            i = ctr["pt"] % NPT
            ctr["pt"] += 1
            return PT[i], BPT[i]

        def mm(bank_i, out_ap, lhsT, rhs, start, stop, reads):
            P.op("pe", lambda e: e.matmul(out_ap, lhsT=lhsT, rhs=rhs, start=start, stop=stop), reads=reads, writes=[BK[bank_i]])

        def tr(bank_i, out_ap, in_ap, reads, ident=None):
            idn = ID[:] if ident is None else ident
            P.op("pe", lambda e: e.transpose(out=out_ap, in_=in_ap, identity=idn), reads=list(reads) + [B["ID"]], writes=[BK[bank_i]])

        def sml(ap):
            n = 1
            for d in ap.shape[1:]:
                n *= d
            return n <= 160

        def act(out_ap, in_ap, func, reads, writes, scale=None, bias=None):
            kw = {}
            if scale is not None:
                kw["scale"] = scale
            if bias is not None:
                kw["bias"] = bias
            P.op("act", lambda e: e.activation(out=out_ap, in_=in_ap, func=func, **kw), reads=reads, writes=writes, small=sml(out_ap))

        def tt(out_ap, in0, in1, op, reads, writes, eng="dve"):
            P.op(eng, lambda e: e.tensor_tensor(out=out_ap, in0=in0, in1=in1, op=op), reads=reads, writes=writes, small=sml(out_ap))

        def ts(out_ap, in0, s1, s2, op0, op1, reads, writes, eng="dve"):
            if s2 is None:
                P.op(eng, lambda e: e.tensor_scalar(out=out_ap, in0=in0, scalar1=s1, scalar2=None, op0=op0), reads=reads, writes=writes, small=sml(out_ap))
            else:
                P.op(eng, lambda e: e.tensor_scalar(out=out_ap, in0=in0, scalar1=s1, scalar2=s2, op0=op0, op1=op1), reads=reads, writes=writes, small=sml(out_ap))

        def stt(out_ap, in0, scalar, in1, op0, op1, reads, writes, eng="dve"):
            P.op(eng, lambda e: e.scalar_tensor_tensor(out=out_ap, in0=in0, scalar=scalar, in1=in1, op0=op0, op1=op1), reads=reads, writes=writes, small=sml(out_ap))

        def cp(out_ap, in_ap, reads, writes, eng="dve"):
            if eng == "act":
                act(out_ap, in_ap, AF.Copy, reads, writes)
            else:
                P.op(eng, lambda e: e.tensor_copy(out=out_ap, in_=in_ap), reads=reads, writes=writes, small=sml(out_ap))

        def ld(eng, out_ap, in_ap, slot, writes, reads=()):
            P.dma(eng, lambda e: e.dma_start(out=out_ap, in_=in_ap), slot, reads=reads, writes=writes)

        def ld_nc(eng, out_ap, in_ap, slot, writes, reads=()):
            P.dma(eng, lambda e: e.dma_start(out=out_ap, in_=in_ap, allow_slow_non_contiguous=True), slot, reads=reads, writes=writes)

        def dbg_store(name, src_ap, srcbufs):
            if name not in dbg_d:
                return
            b = BDBG.setdefault(name, Buf("dbg_" + name))
            P.dma("pool", lambda e: e.dma_start(out=dbg_d[name], in_=src_ap), "dbg_" + name, reads=list(srcbufs), writes=[b])

        def rstd(out_ap, var_ap, bufs, mul=None):
            if mul is None:
                ts(out_ap, var_ap, LN_EPS, None, ALU.add, None, bufs, bufs)
            else:
                ts(out_ap, var_ap, mul, LN_EPS, ALU.mult, ALU.add, bufs, bufs)
            act(out_ap, out_ap, AF.Ln, bufs, bufs)
            act(out_ap, out_ap, AF.Exp, bufs, bufs, scale=-0.5)

        def evac_eng():
            ctr["ev"] += 1
            return "act" if ctr["ev"] % 2 else "dve"

        ld("sp", IDf[:], dr["c_ident"], "c0", [B["IDf"]])
        cp(ID[:], IDf[:], [B["IDf"]], [B["ID"]])
        ld("sp", MNf[:], dr["c_maskneg"], "c1", [B["MNf"]])
        cp(MN[:], MNf[:], [B["MNf"]], [B["MN"]])
        ld("sp", TRIL[:], dr["c_tril"], "c2", [B["TRIL"]])
        ld("sp", PERMf[:], dr["c_perm"], "c3", [B["PERMf"]])
        cp(PERM[:], PERMf[:], [B["PERMf"]], [B["PERM"]])
        ld("sp", SELf[:], dr["c_sel"], "c4", [B["SELf"]])
        P.op("dve", lambda e: e.memset(SEL[:], 0.0), writes=[B["SEL"]])
        P.op("dve", lambda e: e.memset(GTT[:], 0.0), writes=[B["GTT"]])
        cp(SEL[0:16], SELf[:].unsqueeze(2).to_broadcast([16, 16, 128]), [B["SELf"]], [B["SEL"]])
        P.op("dve", lambda e: e.memset(ZER[:], 0.0), writes=[B["ZER"]])
        P.op("dve", lambda e: e.memset(QRZ[:], 0.0), writes=[B["QRZ"]])
        P.op("dve", lambda e: e.memset(KC[64:96, 2, :], 0.0), writes=BKC)
        P.op("dve", lambda e: e.memset(VC[:], 1.0), writes=[B["VC"]])

        def load_layer_params(l):
            tpw, bpw = tmp()
            PWf = tpw[:, 0:256].rearrange("p (c n) -> p c n", c=2)
            P.op("dve", lambda e: e.memset(PWf, 0.0), writes=[bpw])
            for g in range(4):
                c, hh = g // 2, g % 2
                ld("sp", PWf[hh * 64:(hh + 1) * 64, c, hh * 64:(hh + 1) * 64], dr["pool_w"][l, g], "pw%d" % g, [bpw])
            cp(PW[:], PWf, [bpw], [B["PW"]])
            ld_nc("sp", PSC[:], dr["pool_scale"][l].rearrange("(c p) -> p c", p=128), "psc", [B["PSC"]])
            for j in range(3):
                ld_nc("sp", CW[:, :, j], dr["conv_w"][l, j].rearrange("(c p) -> p c", p=128), "cw%d" % j, [B["CW"]])
            for i, nm in enumerate(["lam_q1", "lam_k1", "lam_q2", "lam_k2"]):
                ld("sp", LAMV[:, i, :], dr[nm][l:l + 1, :].partition_broadcast(128) if False else dr[nm][l].partition_broadcast(128), "lam%d" % i, [B["LAMV"]])
            lam_init = 0.8 - 0.6 * math.exp(-0.3 * l)
            tt(LAMT[:, 0:1].unsqueeze(2).to_broadcast([128, 1, 32]) if False else SM[:, 0:32], LAMV[:, 0, :], LAMV[:, 1, :], ALU.mult, [B["LAMV"]], [B["SM"]])
            P.op("dve", lambda e: e.tensor_reduce(out=LAMT[:, 0:1], in_=SM[:, 0:32], axis=AX.X, op=ALU.add), small=True, reads=[B["SM"]], writes=[B["LAMT"]])
            tt(SM[:, 32:64], LAMV[:, 2, :], LAMV[:, 3, :], ALU.mult, [B["LAMV"]], [B["SM"]])
            P.op("dve", lambda e: e.tensor_reduce(out=LAMT[:, 1:2], in_=SM[:, 32:64], axis=AX.X, op=ALU.add), small=True, reads=[B["SM"]], writes=[B["LAMT"]])
            act(LAMT[:, 2:4], LAMT[:, 0:2], AF.Exp, [B["LAMT"]], [B["LAMT"]])
            stt(LAMT[:, 4:5], LAMT[:, 3:4], -lam_init, LAMT[:, 2:3], ALU.add, ALU.subtract, [B["LAMT"]], [B["LAMT"]])
            ld("sp", SUBG[:], dr["subln_g"][l].partition_broadcast(128), "subg", [B["SUBG"]])
            ts(SUBG[:], SUBG[:], 1.0 - lam_init, None, ALU.mult, None, [B["SUBG"]], [B["SUBG"]])
            ld("sp", SGG[:], dr["sg_ln_g"][l].partition_broadcast(128), "sgg", [B["SGG"]])
            ld("sp", SGBb[:], dr["sg_ln_b"][l].partition_broadcast(128), "sgbb", [B["SGBb"]])
            ld("pool", SGWn[:], dr["sg_w"][l].rearrange("g t s -> t g s"), "sgwn", [B["SGWn"]])
            for g in range(4):
                tr(g % 2, pbf(g % 2)[:, 0:128], SGWn[:, g, :], [B["SGWn"]])
                tt(SGW[:, g, :], pbf(g % 2)[:, 0:128], TRIL[:], ALU.mult, [BK[g % 2], B["TRIL"]], [B["SGW"]])
            ld_nc("sp", SGB[:], dr["sg_b"][l].rearrange("g t -> t g"), "sgb", [B["SGB"]])
            ld_nc("pool", WR[:, :, 0:4], dr["w_rg"][l].rearrange("(k p) g -> p k g", p=128), "wr0", [B["WR"]])
            for g in range(4):
                ld_nc("pool", WR[:, :, 4 + 4 * g:8 + 4 * g], dr["w_re"][l, g].rearrange("(k p) e -> p k e", p=128), "wr%d" % (g + 1), [B["WR"]])
            ld("sp", BR[:, 0:4], dr["b_rg"][l].partition_broadcast(128), "br0", [B["BR"]])
            ld("sp", BR[:, 4:20], dr["b_re"][l].rearrange("g e -> (g e)").partition_broadcast(128), "br1", [B["BR"]])

        def xT_prep(s):
            tb, bb = tmp()
            xb = tb[:].bitcast(BF16)[:, 0:1024]
            cp(xb, X[:, s, :], [B["X"]], [bb], eng="act")
            return xb, bb

        def xT_sub(s, from_xn, prep=None, evac=None):
            bi = s % 2
            if from_xn:
                for c in range(8):
                    tr(bi, pbf(bi)[:, c * 128:(c + 1) * 128], XN[:, s, c * 128:(c + 1) * 128], [B["XN"]])
            else:
                xb, bb = prep if prep is not None else xT_prep(s)
                for c in range(8):
                    tr(bi, pbf(bi)[:, c * 128:(c + 1) * 128], xb[:, c * 128:(c + 1) * 128], [bb])
            cp(XT[:, :, s * 128:(s + 1) * 128], pbf(bi).rearrange("p (c t) -> p c t", c=8), [BK[bi]], [B["XT"]],
               eng=(evac if evac is not None else ("dve" if s % 2 == 0 else "act")))

        def prefetch_xn(l, t):
            src = dr["x"] if l == 0 else xs_d
            rd = [] if l == 0 else [BXS[t]]
            ld("pool", XN[:], src[t * T:(t + 1) * T, :].rearrange("(s p) d -> p s d", p=128), "xn", [B["XN"]], reads=rd)

        scr_ids = {}
        BSCR = {}
        USE_SCR = bool(int(os.environ.get("K_SCR", "1")))

        def wld(key, dst_ap, src_ap, slot_sem, slot_buf):
            if not USE_SCR:
                ld("pool", dst_ap, src_ap, slot_sem, [slot_buf])
                return
            k_, n_ = dst_ap.shape[1], dst_ap.shape[2]
            first = key not in scr_ids
            if first:
                scr_ids[key] = len(scr_ids)
                BSCR[key] = Buf("scr%d" % scr_ids[key])
            view = wscr[scr_ids[key]][:, 0:k_ * n_].rearrange("p (k n) -> p k n", k=k_)
            if first:
                ld("pool", dst_ap, src_ap, slot_sem, [slot_buf])
                if NT > 1:
                    P.dma("sp", lambda e: e.dma_start(out=view, in_=dst_ap), "st_" + slot_sem, reads=[slot_buf], writes=[BSCR[key]])
            else:
                P.dma("sp", lambda e: e.dma_start(out=dst_ap, in_=view), slot_sem, reads=[BSCR[key]], writes=[slot_buf])

        def load_w_in_block(l, c0, width):
            i = wslot()
            wld(("w_in", l, c0), WS[i][:, :, 0:width], dr["w_in"][l, :, c0:c0 + width].rearrange("(k p) n -> p k n", p=128), "ws%d" % i, BWS[i])
            return i

        pre = {}

        bankrot = {"i": 0}

        def nbank(lo=0, hi=8):
            n = hi - lo
            b = lo + bankrot["i"] % n
            bankrot["i"] += 1
            return b

        def proj_fm(wi, col, width, bank_i):
            for k in range(8):
                mm(bank_i, PB[bank_i][0:width, :], WS[wi][:, k, col:col + width], XT[:, k, :], k == 0, k == 7, [BWS[wi], B["XT"]])

        def mixer_head(l, t):
            ld("sp", COS[:], dr["c_cos"][:, t * T:(t + 1) * T], "cos", [B["COS"]])
            ld("sp", SIN[:], dr["c_sin"][:, t * T:(t + 1) * T], "sin", [B["SIN"]])
            if t <= 1:
                ld("sp", RC[:], dr["c_rc"][min(t, 1)], "rc", [B["RC"]])
            for s_ in range(4):
                xT_sub(s_, True, evac="act")
            if t + 1 < NT:
                prefetch_xn(l, t + 1)
            elif l + 1 < NL:
                prefetch_xn(l + 1, 0)

        def mixer(l, t):
            lam_init = 0.8 - 0.6 * math.exp(-0.3 * l)
            src = dr["x"] if l == 0 else xs_d
            rd = [] if l == 0 else [BXS[t]]
            if not pre.pop("head", False):
                mixer_head(l, t)

            if "w0" in pre:
                w0, w1 = pre.pop("w0"), pre.pop("w1")
            else:
                w0 = load_w_in_block(l, 0, 512)
                w1 = load_w_in_block(l, 512, 512)
            GB = []
            for cc in range(4):
                bi = nbank()
                proj_fm(w0, cc * 128, 128, bi)
                if cc < 2:
                    A = A_[cc]
                    BA = B["A%d" % cc]
                    if t == 0:
                        P.op("act", lambda e, A=A: e.memset(A[:, 0:16], 0.0) if False else e.activation(out=A[:, 0:16], in_=ZER[:, 0:16], func=AF.Copy), reads=[B["ZER"]], writes=[BA])
                    else:
                        act(A[:, 0:16], A[:, 512:528], AF.Copy, [BA], [BA])
                    act(A[:, 16:528], PB[bi][:], AF.Copy, [BK[bi]], [BA])
                else:
                    tg, bg = tmp()
                    act(tg[:, 0:512], PB[bi][:], AF.Copy, [BK[bi]], [bg])
                    GB.append((tg, bg))
            for c in range(2):
                bgc = nbank()
                proj_fm(w1, c * 128, 128, bgc)
                bh = nbank()
                proj_fm(w1, 256 + c * 128, 128, bh)
                tg, bg = tmp()
                act(tg[:, 0:512], PB[bgc][:], AF.Copy, [BK[bgc]], [bg])
                Z = Z_[c]
                BZ = B["Z%d" % c]
                if t == 0:
                    cp(Z[:, 0:2], ZER[:, 0:2], [B["ZER"]], [BZ])
                else:
                    cp(Z[:, 0:2], Z[:, 512:514], [BZ], [BZ])
                tt(Z[:, 2:514], tg[:, 0:512], PB[bh][:], ALU.mult, [bg, BK[bh]], [BZ])
                ta, ba = tmp()
                ts(ta[:, 0:512], Z[:, 0:512], CW[:, c, 0:1], None, ALU.mult, None, [BZ, B["CW"]], [ba])
                stt(ta[:, 0:512], Z[:, 1:513], CW[:, c, 1:2], ta[:, 0:512], ALU.mult, ALU.add, [BZ, B["CW"], ba], [ba])
                stt(ta[:, 0:512], Z[:, 2:514], CW[:, c, 2:3], ta[:, 0:512], ALU.mult, ALU.add, [BZ, B["CW"], ba], [ba])
                gbt, gbb = GB[c]
                tt(YT[1][:, c, :], ta[:, 0:512], gbt[:, 0:512], ALU.mult, [ba, gbb], bYT(1))
            w2 = pre.pop("w2") if "w2" in pre else load_w_in_block(l, 1024, 512)

            def qk_proj(qk, r):
                wd_ = 96 if r < 2 else 64
                col = qk * 256 + 96 * r
                bi = nbank()
                proj_fm(w2, col, wd_, bi)
                tq, bq = tmp()
                qs = tq[:].bitcast(BF16)[0:wd_, 0:512]
                act(qs, PB[bi][0:wd_, :], AF.Copy, [BK[bi]], [bq])
                return (qk, r, wd_, bi, qs, bq)

            def qk_rot(st_):
                qk, r, wd_, bi, qs, bq = st_
                b2 = nbank()
                mm(b2, PB[b2][0:wd_, :], PERM[0:wd_, 0:wd_], qs, True, True, [B["PERM"], bq])
                t1, bt1 = tmp()
                t2, bt2 = tmp()
                tt(t1[0:wd_, 0:512], PB[bi][0:wd_, :], COS[0:wd_, :], ALU.mult, [BK[bi], B["COS"]], [bt1])
                tt(t2[0:wd_, 0:512], PB[b2][0:wd_, :], SIN[0:wd_, :], ALU.mult, [BK[b2], B["SIN"]], [bt2])
                if qk == 0:
                    for pl in range(wd_ // 32):
                        tt(QRZ[32 * pl:32 * pl + 32, 3 * r + pl, :], t1[32 * pl:32 * pl + 32, 0:512], t2[32 * pl:32 * pl + 32, 0:512], ALU.add,
                           [bt1, bt2], [B["QRZ"]])
                else:
                    tt(KC[0:wd_, r, t * T:(t + 1) * T], t1[0:wd_, 0:512], t2[0:wd_, 0:512], ALU.add, [bt1, bt2], [BKC[t]])

            qkitems = [(qk, r) for qk in range(2) for r in range(3)]
            prev = None
            for (qk, r) in qkitems:
                cur = qk_proj(qk, r)
                if prev is not None:
                    qk_rot(prev)
                prev = cur
            qk_rot(prev)

            w3 = load_w_in_block(l, 1536, 512)
            w4 = load_w_in_block(l, 2048, 256)
            def sg_proj(s):
                b1 = nbank()
                for k in range(8):
                    mm(b1, PB[b1][:], XT[:, k, s * 128:(s + 1) * 128], WS[w3][:, k, :], k == 0, k == 7, [B["XT"], BWS[w3]])
                b2 = nbank()
                for k in range(8):
                    mm(b2, PB[b2][:, 0:256], XT[:, k, s * 128:(s + 1) * 128], WS[w4][:, k, 0:256], k == 0, k == 7, [B["XT"], BWS[w4]])
                act(VC[:, t * 4 + s, :, 0:64], PB[b1][:, 0:256].rearrange("p (h e) -> p h e", h=4), AF.Copy, [BK[b1]], [B["VC"]])
                tu, bu = tmp()
                act(tu[:, 0:256], PB[b1][:, 256:512], AF.Gelu_apprx_tanh, [BK[b1]], [bu])
                tv, bv = tmp()
                act(tv[:, 0:256], PB[b2][:, 0:256], AF.Gelu_apprx_tanh, [BK[b2]], [bv])
                so = (s % 2) * 16
                P.op("dve", lambda e, tv=tv, so=so: e.bn_stats(out=SM[:, 64 + so:70 + so], in_=tv[:, 0:256]), small=True, reads=[bv], writes=[B["SM"]])
                P.op("dve", lambda e, so=so: e.bn_aggr(out=SM[:, 72 + so:74 + so], in_=SM[:, 64 + so:70 + so]), small=True, reads=[B["SM"]], writes=[B["SM"]])
                rstd(SM[:, 74 + so:75 + so], SM[:, 73 + so:74 + so], [B["SM"]])
                ts(tv[:, 0:256], tv[:, 0:256], SM[:, 72 + so:73 + so], SM[:, 74 + so:75 + so], ALU.subtract, ALU.mult, [bv, B["SM"]], [bv])
                tt(tv[:, 0:256], tv[:, 0:256], SGG[:], ALU.mult, [bv, B["SGG"]], [bv])
                vn = tv[:].bitcast(BF16)[:, 512:768]
                tt(vn, tv[:, 0:256], SGBb[:], ALU.add, [bv, B["SGBb"]], [bv])
                return tu, bu, vn, bv

            def sg_spatial(s, st_):
                tu, bu, vn, bv = st_
                b3 = nbank()
                for g in range(4):
                    mm(b3, PB[b3][:, g * 64:(g + 1) * 64], SGW[:, g, :], vn[:, g * 64:(g + 1) * 64], True, True, [B["SGW"], bv])
                tt(tu[:, 256:512].rearrange("p (g c) -> p g c", g=4), PB[b3][:, 0:256].rearrange("p (g c) -> p g c", g=4),
                   SGB[:].unsqueeze(2).to_broadcast([128, 4, 64]), ALU.add, [BK[b3], B["SGB"], bu], [bu])
                tt(YD[:, s, :], tu[:, 256:512], tu[:, 0:256], ALU.mult, [bu], bYD)

            sgst = {}
            for s in range(4):
                sgst[s] = sg_proj(s)
                if s >= 1:
                    sg_spatial(s - 1, sgst.pop(s - 1))
            sg_spatial(3, sgst.pop(3))
            bi = nbank(0, 2)
            for s in range(4):
                for c in range(2):
                    tr(bi, pbf(bi)[:, c * 512 + s * 128:c * 512 + (s + 1) * 128], YD[:, s, c * 128:(c + 1) * 128], bYD)
            cp(YT[3][:].rearrange("p c t -> p (c t)"), pbf(bi), [BK[bi]], bYT(3), eng="act")

            pool_pend = []

            def emit_pool():
                for c in range(2):
                    A = A_[c]
                    BA = B["A%d" % c]
                    sc, bsc = tmp()
                    ta, ba = tmp()
                    if c == 0:
                        tt(ta[64:128, 1:528], A[64:128, 1:528], A[64:128, 0:527], ALU.add, [BA], [ba])
                        tt(sc[64:128, 0:512], ta[64:128, 16:528], ta[64:128, 14:526], ALU.add, [ba], [bsc])
                        tt(sc[0:64, 0:512], A[0:64, 16:528], A[0:64, 15:527], ALU.add, [BA], [bsc])
                    else:
                        tb_, bb_ = tmp()
                        tt(ta[:, 1:528], A[:, 1:528], A[:, 0:527], ALU.add, [BA], [ba])
                        tt(tb_[:, 3:528], ta[:, 3:528], ta[:, 1:526], ALU.add, [ba], [bb_])
                        tt(sc[0:64, 0:512], tb_[0:64, 16:528], tb_[0:64, 12:524], ALU.add, [bb_], [bsc])
                        tt(ta[64:128, 7:528], tb_[64:128, 7:528], tb_[64:128, 3:524], ALU.add, [bb_, ba], [ba])
                        tt(sc[64:128, 0:512], ta[64:128, 16:528], ta[64:128, 8:520], ALU.add, [ba], [bsc])
                    tt(sc[:, 0:512], sc[:, 0:512], RC[:, c, :], ALU.mult, [bsc, B["RC"]], [bsc])
                    dT = DTP[:, c, :]
                    bd = B["DTP%d" % c]
                    tt(dT, sc[:, 0:512], A[:, 16:528], ALU.subtract, [bsc, BA], [bd])
                    pool_pend.append((c, dT, bd))

            def emit_pool_mm():
                for (c, dT, bd) in pool_pend:
                    bi = 3
                    mm(bi, PB[bi][:], PW[:, c, :], dT, True, True, [B["PW"], bd])
                    ts(YT[0][:, c, :], PB[bi][:], PSC[:, c:c + 1], None, ALU.mult, None, [BK[bi], B["PSC"]], bYT(0))


            nkb = 4 * t + 4
            items = [(h, kb, m) for h in range(4) for kb in range(nkb) for m in range(2)]
            st_info = {}

            def issue_st(idx):
                h, kb, m = items[idx]
                diag = kb >= 4 * t
                j = kb - 4 * t if diag else 0
                qlo = j * 128
                n = T - qlo
                pair = 2 * h + m
                r = pair // 3
                sbk = idx % 3
                mm(sbk, PB[sbk][:, 0:n], KC[0:96, r, kb * 128:(kb + 1) * 128], QRZ[0:96, pair, qlo:T], True, not diag,
                   [BKC[kb // 4], B["QRZ"]])
                if diag:
                    mm(sbk, PB[sbk][:, 0:128], ID[:], MN[:], False, True, [B["ID"], B["MN"]])
                st_info[idx] = (sbk, j, n)

            def head_banks(h):
                return [4 + 2 * (h % 2), 5 + 2 * (h % 2)]

            def finish_head(h):
                ob = head_banks(h)
                o1 = PB[ob[0]][:, 0:260].rearrange("p (q e) -> p q e", q=4)
                o2 = PB[ob[1]][:, 0:260].rearrange("p (q e) -> p q e", q=4)
                so = 128 + h * 16
                P.op("dve", lambda e, o1=o1, so=so: e.reciprocal(out=SM[:, so:so + 4], in_=o1[:, :, 64]), small=True, reads=[BK[ob[0]]], writes=[B["SM"]])
                P.op("dve", lambda e, o2=o2, so=so: e.reciprocal(out=SM[:, so + 4:so + 8], in_=o2[:, :, 64]), small=True, reads=[BK[ob[1]]], writes=[B["SM"]])
                ta, ba = tmp()
                tb_, bb_ = tmp()
                av = ta[:, 0:256].rearrange("p (q e) -> p q e", q=4)
                bv_ = tb_[:, 0:256].rearrange("p (q e) -> p q e", q=4)
                tt(av, o1[:, :, 0:64], SM[:, so:so + 4].unsqueeze(2).to_broadcast([128, 4, 64]), ALU.mult, [BK[ob[0]], B["SM"]], [ba])
                tt(bv_, o2[:, :, 0:64], SM[:, so + 4:so + 8].unsqueeze(2).to_broadcast([128, 4, 64]), ALU.mult, [BK[ob[1]], B["SM"]], [bb_])
                stt(ta[:, 0:256], tb_[:, 0:256], LAMT[:, 4:5], ta[:, 0:256], ALU.mult, ALU.add, [bb_, B["LAMT"], ba], [ba])
                tt(tb_[:, 0:256], ta[:, 0:256], ta[:, 0:256], ALU.mult, [ba], [bb_])
                P.op("dve", lambda e, bv_=bv_, so=so: e.tensor_reduce(out=SM[:, so + 8:so + 12], in_=bv_, axis=AX.X, op=ALU.add), small=True, reads=[bb_], writes=[B["SM"]])
                rstd(SM[:, so + 8:so + 12], SM[:, so + 8:so + 12], [B["SM"]], mul=1.0 / 64.0)
                tt(av, av, SM[:, so + 8:so + 12].unsqueeze(2).to_broadcast([128, 4, 64]), ALU.mult, [ba, B["SM"]], [ba])
                tt(YC[:, :, h * 64:(h + 1) * 64], av, SUBG[:].unsqueeze(1).to_broadcast([128, 4, 64]), ALU.mult, [ba, B["SUBG"]], bYC)

            LOOK = 2
            for idx in range(min(LOOK, len(items))):
                issue_st(idx)
            for idx, (h, kb, m) in enumerate(items):
                if idx == 0:
                    emit_pool()
                if idx == len(items) // 2:
                    emit_pool_mm()
                ob = head_banks(h)
                if kb == 0 and m == 0:
                    for mm_ in range(2):
                        mm(ob[mm_], PB[ob[mm_]][:], ZER[:, 0:128], ZER[:], True, False, [B["ZER"]])
                if idx + LOOK < len(items):
                    issue_st(idx + LOOK)
                sbk, j, n = st_info.pop(idx)
                pt, bpt = ptslot()
                act(pt[:, 0:n], PB[sbk][:, 0:n], AF.Exp, [BK[sbk]], [bpt], scale=SCALE)
                last = (kb == nkb - 1)
                for qs in range(j, 4):
                    P.op("pe", lambda e, ob=ob, m=m, qs=qs, pt=pt, j=j, kb=kb, h=h, last=last: e.matmul(
                        PB[ob[m]][:, qs * 65:(qs + 1) * 65], lhsT=pt[:, (qs - j) * 128:(qs - j + 1) * 128],
                        rhs=VC[:, kb, h, :], start=False, stop=last, skip_group_check=True),
                        reads=[bpt, B["VC"]], writes=[BK[ob[m]]])
                if last and m == 1:
                    finish_head(h)
            bi = nbank(0, 2)
            for s in range(4):
                for c in range(2):
                    tr(bi, pbf(bi)[:, c * 512 + s * 128:c * 512 + (s + 1) * 128], YC[:, s, c * 128:(c + 1) * 128], bYC)
            cp(YT[2][:].rearrange("p c t -> p (c t)"), pbf(bi), [BK[bi]], bYT(2), eng="act")

            if l == 0 and t == 0:
                dbg_store("xt", XT[:], [B["XT"]])
                for i in range(4):
                    dbg_store("yt%d" % i, YT[i], bYT(i))
            ld("sp", X[:], src[t * T:(t + 1) * T, :].rearrange("(s p) d -> p s d", p=128), "xload", [B["X"]], reads=rd)

            border = [1, 3, 0, 2]
            for i in border:
                for hb in range(2):
                    wb = wdslot()
                    wld(("wbr", l, i, hb), WD[wb][:], dr["w_branch"][l, i, :, hb * 512:(hb + 1) * 512].rearrange("(k p) n -> p k n", p=128), "wd%d" % wb, BWD[wb])
                    wg = load_w_in_block(l, 2304 + i * 1024 + hb * 512, 512)
                    for jj in range(4):
                        j = hb * 4 + jj
                        bg_ = nbank()
                        proj_fm(wg, jj * 128, 128, bg_)
                        bb2 = nbank()
                        for k2 in range(2):
                            mm(bb2, PB[bb2][:], WD[wb][:, k2, jj * 128:(jj + 1) * 128], YT[i][:, k2, :], k2 == 0, k2 == 1, [BWD[wb]] + bYT(i))
                        tsg, bsg = tmp()
                        act(tsg[:, 0:512], PB[bg_][:], AF.Sigmoid, [BK[bg_]], [bsg])
                        if i == border[0]:
                            tt(MACC[:, j, :], tsg[:, 0:512], PB[bb2][:], ALU.mult, [bsg, BK[bb2]], bMACC(j))
                        else:
                            tt(tsg[:, 0:512], tsg[:, 0:512], PB[bb2][:], ALU.mult, [bsg, BK[bb2]], [bsg])
                            if i != border[-1]:
                                tt(MACC[:, j, :], MACC[:, j, :], tsg[:, 0:512], ALU.add, bMACC(j) + [bsg], bMACC(j))
                            else:
                                tt(MT[:, j, :], MACC[:, j, :], tsg[:, 0:512], ALU.add, bMACC(j) + [bsg], bMT(j))

            ld("sp", LNG[:], dr["ln1_g"][l].partition_broadcast(128), "lng", [B["LNG"]])
            ld("sp", LNB[:], dr["ln1_b"][l].partition_broadcast(128), "lnb", [B["LNB"]])
            wos = []
            for hf in range(2):
                wo = wslot()
                wld(("wo", l, hf), WS[wo][:], dr["w_o"][l, :, hf * 512:(hf + 1) * 512].rearrange("(k p) n -> p k n", p=128), "ws%d" % wo, BWS[wo])
                wos.append(wo)
            preps = []
            for s in range(4):
                for hf in range(2):
                    wo = wos[hf]
                    bi = nbank(2, 8)
                    for j in range(8):
                        mm(bi, PB[bi][:], MT[:, j, s * 128:(s + 1) * 128], WS[wo][:, j, :], j == 0, j == 7, bMT(j) + [BWS[wo]])
                    stt(X[:, s, hf * 512:(hf + 1) * 512], X[:, s, hf * 512:(hf + 1) * 512], ALPHA, PB[bi][:], ALU.mult, ALU.add, [B["X"], BK[bi]], [B["X"]])
                if l == 0 and t == 0 and s == 3:
                    dbg_store("mt", MT, bMTall)
                layer_norm_s(s)
                preps.append(xT_prep(s))
                if s >= 1:
                    xT_sub(s - 1, False, preps[s - 1])
            xT_sub(3, False, preps[3])
            if l == 0 and t == 0:
                dbg_store("x1", X[:], [B["X"]])

        def layer_norm_inplace():
            for s in range(4):
                layer_norm_s(s)

        def layer_norm_s(s):
            if True:
                so = 256 + s * 32
                for hf in range(2):
                    P.op("dve", lambda e, s=s, hf=hf, so=so: e.bn_stats(out=SM[:, so + 6 * hf:so + 6 * hf + 6], in_=X[:, s, hf * 512:(hf + 1) * 512]),
                         small=True, reads=[B["X"]], writes=[B["SM"]])
                P.op("dve", lambda e, so=so: e.bn_aggr(out=SM[:, so + 12:so + 14], in_=SM[:, so:so + 12]), small=True, reads=[B["SM"]], writes=[B["SM"]])
                rstd(SM[:, so + 14:so + 15], SM[:, so + 13:so + 14], [B["SM"]])
                ts(X[:, s, :], X[:, s, :], SM[:, so + 12:so + 13], SM[:, so + 14:so + 15], ALU.subtract, ALU.mult, [B["X"], B["SM"]], [B["X"]])
                lne = os.environ.get("K_LNENG", "pool")
                tt(X[:, s, :], X[:, s, :], LNG[:], ALU.mult, [B["X"], B["LNG"]], [B["X"]], eng=lne)
                tt(X[:, s, :], X[:, s, :], LNB[:], ALU.add, [B["X"], B["LNB"]], [B["X"]], eng=lne)

        def moe(l, t, last_layer):
            rb = nbank(0, 4)
            for s in range(4):
                for k in range(8):
                    mm(rb, PB[rb][:, s * 32:s * 32 + 20], XT[:, k, s * 128:(s + 1) * 128], WR[:, k, :], k == 0, k == 7, [B["XT"], B["WR"]])
            LG = SM[:, 512:592].rearrange("p (s c) -> p s c", s=4)
            tt(LG, PB[rb][:, 0:128].rearrange("p (s c) -> p s c", s=4)[:, :, 0:20], BR[:].unsqueeze(1).to_broadcast([128, 4, 20]), ALU.add,
               [BK[rb], B["BR"]], [B["SM"]])
            gl = LG[:, :, 0:4]
            el = LG[:, :, 4:20].rearrange("p s (g e) -> p s g e", g=4)
            o = 600
            GM = SM[:, o:o + 4]
            OHG = SM[:, o + 4:o + 20].rearrange("p (s g) -> p s g", s=4)
            EG = SM[:, o + 20:o + 36].rearrange("p (s g) -> p s g", s=4)
            SG_ = SM[:, o + 36:o + 40]
            PSEL = SM[:, o + 40:o + 44]
            T4 = SM[:, o + 44:o + 108].rearrange("p (s g e) -> p s g e", s=4, g=4)
            ELS = SM[:, o + 108:o + 124].rearrange("p (s e) -> p s e", s=4)
            M1 = SM[:, o + 124:o + 128]
            OH1 = SM[:, o + 128:o + 144].rearrange("p (s e) -> p s e", s=4)
            MSK = SM[:, o + 144:o + 160].rearrange("p (s e) -> p s e", s=4)
            M2_ = SM[:, o + 160:o + 164]
            OH2 = SM[:, o + 164:o + 180].rearrange("p (s e) -> p s e", s=4)
            W1 = SM[:, o + 180:o + 184]
            W2 = SM[:, o + 184:o + 188]
            GIG = SM[:, o + 188:o + 204].rearrange("p (s e) -> p s e", s=4)
            GIG2 = SM[:, o + 204:o + 220].rearrange("p (s e) -> p s e", s=4)
            GATE = SM[:, o + 220:o + 252].bitcast(BF16).rearrange("p (s g e) -> p s g e", s=4, g=4)
            sm = [B["SM"]]

            def bc3(ap4):
                return ap4.unsqueeze(2).to_broadcast([128, 4, 4])

            P.op("dve", lambda e: e.tensor_reduce(out=GM, in_=gl, axis=AX.X, op=ALU.max), small=True, reads=sm, writes=sm)
            tt(OHG, gl, bc3(GM), ALU.is_equal, sm, sm)
            tt(EG, gl, bc3(GM), ALU.subtract, sm, sm)
            act(EG, EG, AF.Exp, sm, sm)
            P.op("dve", lambda e: e.tensor_reduce(out=SG_, in_=EG, axis=AX.X, op=ALU.add), small=True, reads=sm, writes=sm)
            P.op("dve", lambda e: e.reciprocal(out=PSEL, in_=SG_), small=True, reads=sm, writes=sm)
            tt(T4, el, OHG.unsqueeze(3).to_broadcast([128, 4, 4, 4]), ALU.mult, sm, sm)
            P.op("dve", lambda e: e.tensor_reduce(out=ELS, in_=T4.rearrange("p s g e -> p s e g"), axis=AX.X, op=ALU.add), small=True, reads=sm, writes=sm)
            P.op("dve", lambda e: e.tensor_reduce(out=M1, in_=ELS, axis=AX.X, op=ALU.max), small=True, reads=sm, writes=sm)
            tt(OH1, ELS, bc3(M1), ALU.is_equal, sm, sm)
            stt(MSK, OH1, -1e30, ELS, ALU.mult, ALU.add, sm, sm)
            P.op("dve", lambda e: e.tensor_reduce(out=M2_, in_=MSK, axis=AX.X, op=ALU.max), small=True, reads=sm, writes=sm)
            tt(OH2, MSK, bc3(M2_), ALU.is_equal, sm, sm)
            tt(W1, M1, M2_, ALU.subtract, sm, sm)
            act(W1, W1, AF.Sigmoid, sm, sm)
            tt(W1, W1, PSEL, ALU.mult, sm, sm)
            tt(W2, PSEL, W1, ALU.subtract, sm, sm)
            tt(GIG, OH1, bc3(W1), ALU.mult, sm, sm)
            tt(GIG2, OH2, bc3(W2), ALU.mult, sm, sm)
            tt(GIG, GIG, GIG2, ALU.add, sm, sm)
            tt(GATE, OHG.unsqueeze(3).to_broadcast([128, 4, 4, 4]), GIG.unsqueeze(2).to_broadcast([128, 4, 4, 4]), ALU.mult, sm, sm)
            if l == 0 and t == 0:
                dbg_store("sm", SM[:], sm)
            bgt = B["GTT"]
            GT = GTT[:]

            def gate_T():
                gb_ = nbank()
                for s in range(4):
                    tr(gb_, pbf(gb_)[0:16, s * 128:(s + 1) * 128], GATE[:, s].rearrange("p g e -> p (g e)"), sm)
                cp(GTT[0:16, :], pbf(gb_)[0:16, 0:512], [BK[gb_]], [bgt])

            def m2_expert(e_, hf):
                wd = wdslot()
                wld(("wd", l, e_, hf), WD[wd][:], dr["w_down"][l, e_, :, hf * 512:(hf + 1) * 512].rearrange("(k p) n -> p k n", p=128), "wd%d" % wd, BWD[wd])
                for hc in range(2):
                    for s in range(4):
                        bi = hf * 4 + s
                        mm(bi, PB[bi][:], ACTT[:, e_ * 2 + hc, s * 128:(s + 1) * 128], WD[wd][:, hc, :], (e_ == 0 and hc == 0), (e_ == 15 and hc == 1),
                           [BACT[e_ * 2 + hc], BWD[wd]])

            LAG = 4
            pend = {}
            wcur = {}
            for i in range(32 + LAG):
                if i < 32:
                    e_, hc = i // 2, i % 2
                    if hc == 0:
                        wi = wslot()
                        wld(("wg", l, e_), WS[wi][:, :, 0:256], dr["w_gate"][l, e_].rearrange("(k p) n -> p k n", p=128), "ws%d" % wi, BWS[wi])
                        wld(("wu", l, e_), WS[wi][:, :, 256:512], dr["w_up"][l, e_].rearrange("(k p) n -> p k n", p=128), "ws%d" % wi, BWS[wi])
                        wcur["wi"] = wi
                    wi = wcur["wi"]
                    bg_ = nbank()
                    proj_fm(wi, hc * 128, 128, bg_)
                    bu_ = nbank()
                    proj_fm(wi, 256 + hc * 128, 128, bu_)
                    tsl, bsl = tmp()
                    act(tsl[:, 0:512], PB[bg_][:], AF.Silu, [BK[bg_]], [bsl])
                    tt(tsl[:, 0:512], tsl[:, 0:512], PB[bu_][:], ALU.mult, [bsl, BK[bu_]], [bsl])
                    pend[i] = (tsl, bsl)
                if i == LAG - 1 or (LAG == 0 and i == 0):
                    gate_T()
                if i >= LAG:
                    j = i - LAG
                    e_, hc = j // 2, j % 2
                    tsl, bsl = pend.pop(j)
                    gbk = nbank()
                    mm(gbk, PB[gbk][:], SEL[:, e_, :], GT, True, True, [B["SEL"], bgt])
                    tt(ACTT[:, j, :], tsl[:, 0:512], PB[gbk][:], ALU.mult, [bsl, BK[gbk]], [BACT[j]])
            ld("sp", LNG[:], dr["ln2_g"][l].partition_broadcast(128), "lng", [B["LNG"]])
            ld("sp", LNB[:], dr["ln2_b"][l].partition_broadcast(128), "lnb", [B["LNB"]])
            nxt = (l, t + 1) if t + 1 < NT else ((l + 1, 0) if l + 1 < NL else None)
            for e_ in range(16):
                m2_expert(e_, 0)
                if e_ == 3 and nxt is not None:
                    pre["w0"] = load_w_in_block(nxt[0], 0, 512)
                    pre["w1"] = load_w_in_block(nxt[0], 512, 512)
                    pre["w2"] = load_w_in_block(nxt[0], 1024, 512)
            for s in range(4):
                stt(X[:, s, 0:512], X[:, s, 0:512], ALPHA, PB[s][:], ALU.mult, ALU.add, [B["X"], BK[s]], [B["X"]])
            for e_ in range(16):
                m2_expert(e_, 1)
            if nxt is not None:
                mixer_head(nxt[0], nxt[1])
                pre["head"] = True
            for s in range(4):
                stt(X[:, s, 512:1024], X[:, s, 512:1024], ALPHA, PB[4 + s][:], ALU.mult, ALU.add, [B["X"], BK[4 + s]], [B["X"]])
            if l == 0 and t == 0:
                dbg_store("h2pre", X[:], [B["X"]])
            layer_norm_inplace()
            if last_layer:
                P.dma("pool", lambda e: e.dma_start(out=out_d[t * T:(t + 1) * T, :].rearrange("(s p) d -> p s d", p=128), in_=X[:]), "xstore",
                      reads=[B["X"]], writes=[B["out"]])
            else:
                P.dma("pool", lambda e: e.dma_start(out=xs_d[t * T:(t + 1) * T, :].rearrange("(s p) d -> p s d", p=128), in_=X[:]), "xstore",
                      reads=[B["X"]], writes=[BXS[t]])

        prefetch_xn(0, 0)
        for l in range(NL):
            load_layer_params(l)
            for t in range(NT):
                mixer(l, t)
                moe(l, t, l == NL - 1)
        P.wait_all("pool", [B["out"]] + BXS + list(BDBG.values()))
        P.emit(st)
        build.n_ops = P.n_ops
    return nc


_CONSTS = None


def kernel(**inputs):
    global _CONSTS
    if _CONSTS is None:
        _CONSTS = host_consts()
    NT = int(os.environ.get("K_NT", NTILES))
    NL = int(os.environ.get("K_NL", DEPTH))
    nc = build(NT=NT, NL=NL)
    x = np.ascontiguousarray(np.asarray(inputs["x"], dtype=np.float32))
    shared = {}
    for name, _ in WEIGHT_SPECS:
        shared[name] = np.ascontiguousarray(np.asarray(inputs[name], dtype=np.float32))
    shared.update(_CONSTS)
    in_maps = []
    for c in range(8):
        m = dict(shared)
        m["x"] = x[c]
        in_maps.append(m)
    res = run_bass_kernel_spmd(nc, in_maps, core_ids=list(range(8)))
    out = np.stack([np.asarray(res.results[c]["out"], dtype=np.float32) for c in range(8)], axis=0)
    return out
```

```python
import math
import os
import numpy as np
from contextlib import ExitStack
import concourse.bass as bass
import concourse.mybir as mybir
from concourse.bass_utils import run_bass_kernel_spmd

F32 = mybir.dt.float32
BF16 = mybir.dt.bfloat16
AF = mybir.ActivationFunctionType
ALU = mybir.AluOpType
AX = mybir.AxisListType

S = 4096
D = 1024
T = 512
NTILES = S // T
DEPTH = 2
IN_COLS = 6400
LN_EPS = 1e-5
ALPHA = (2 * DEPTH) ** 0.25
SCALE = 32 ** -0.5
ENGS = ("pe", "act", "dve", "pool", "sp")


class Buf:
    __slots__ = ("name", "lw", "rd", "excl", "lws")

    def __init__(self, name, excl=False):
        self.name = name
        self.lw = None
        self.lws = False
        self.rd = {}
        self.excl = excl


class Prog:
    def __init__(self, nc, same_engine_sync=False):
        self.nc = nc
        self.same = same_engine_sync
        self.streams = {e: [] for e in ENGS}
        self.count = {}
        self.seen = {e: {} for e in ENGS}
        self.semkeys = []
        for e in ENGS:
            self._newsem(("eng", e))
        self.n_ops = 0

    def _newsem(self, key):
        if key not in self.count:
            self.count[key] = 0
            self.semkeys.append(key)

    def _deps(self, eng, reads, writes):
        deps = {}
        me = ("eng", eng)
        same_needed = self.same == 1
        for r in reads:
            if r.lw is not None:
                k, c = r.lw
                if k == me and r.lws:
                    same_needed = True
                if deps.get(k, 0) < c:
                    deps[k] = c
        for w in writes:
            if w.lw is not None:
                k, c = w.lw
                if k == me and w.lws:
                    same_needed = True
                if deps.get(k, 0) < c:
                    deps[k] = c
            for k, c in w.rd.items():
                if deps.get(k, 0) < c:
                    deps[k] = c
        out = []
        seen = self.seen[eng]
        for k, c in deps.items():
            if k == me and (eng == "pe" or not same_needed):
                continue
            if seen.get(k, 0) >= c:
                continue
            seen[k] = c
            out.append((k, c))
        return out

    def op(self, eng, fn, reads=(), writes=(), small=False):
        ex = [r for r in reads if r.excl and r not in writes]
        if ex:
            writes = list(writes) + ex
        for k, c in self._deps(eng, reads, writes):
            self.streams[eng].append(("wait", k, c))
        key = ("eng", eng)
        self.count[key] += 1
        c = self.count[key]
        self.streams[eng].append(("op", fn, key, 1))
        for r in reads:
            r.rd[key] = c
        for w in writes:
            w.lw = (key, c)
            w.lws = small
            w.rd = {}
        self.n_ops += 1

    def dma(self, eng, fn, slot, reads=(), writes=()):
        key = ("dma", slot)
        self._newsem(key)
        for k, c in self._deps(eng, reads, writes):
            self.streams[eng].append(("wait", k, c))
        self.count[key] += 16
        c = self.count[key]
        self.streams[eng].append(("op", fn, key, 16))
        for r in reads:
            r.rd[key] = c
        for w in writes:
            w.lw = (key, c)
            w.lws = False
            w.rd = {}
        self.n_ops += 1

    def wait_all(self, eng, bufs):
        for k, c in self._deps(eng, bufs, bufs):
            self.streams[eng].append(("wait", k, c))

    def barrier(self, engs=("pe", "act", "dve")):
        for e in engs:
            for o in engs:
                if o == e:
                    continue
                k = ("eng", o)
                c = self.count[k]
                if c > 0 and self.seen[e].get(k, 0) < c:
                    self.seen[e][k] = c
                    self.streams[e].append(("wait", k, c))

    def emit(self, stack):
        nc = self.nc
        sems = {}
        for k in self.semkeys:
            sems[k] = stack.enter_context(nc.semaphore(("s_%s_%s" % (k[0], str(k[1]))).replace(" ", "").replace(",", "_").replace("(", "").replace(")", "").replace("'", "")))
        block = stack.enter_context(nc.Block())
        streams = self.streams

        def run(engobj, name):
            for item in streams[name]:
                if item[0] == "wait":
                    engobj.wait_ge(sems[item[1]], item[2])
                else:
                    _, fn, key, inc = item
                    fn(engobj).then_inc(sems[key], inc)

        @block.tensor
        def _(e):
            run(e, "pe")

        @block.scalar
        def _(e):
            run(e, "act")

        @block.vector
        def _(e):
            run(e, "dve")

        @block.gpsimd
        def _(e):
            run(e, "pool")

        @block.sync
        def _(e):
            run(e, "sp")


def host_consts():
    c = {}
    c["c_ident"] = np.eye(128, dtype=np.float32)
    k = np.arange(128)[:, None]
    q = np.arange(128)[None, :]
    c["c_maskneg"] = np.where(q >= k, 0.0, -30000.0).astype(np.float32)
    c["c_tril"] = (k <= q).astype(np.float32)
    perm = np.zeros((96, 96), np.float32)
    for po in range(96):
        d = po % 32
        pi = po + 16 if d < 16 else po - 16
        perm[pi, po] = 1.0
    c["c_perm"] = perm
    c["c_sel"] = np.eye(16, dtype=np.float32)
    half = 16
    inv_freq = (10000.0 ** (-np.arange(half, dtype=np.float32) / half)).astype(np.float32)
    ang = np.arange(S, dtype=np.float32)[:, None] * inv_freq[None, :]
    cos = np.cos(ang).astype(np.float32)
    sin = np.sin(ang).astype(np.float32)
    cosT = np.zeros((96, S), np.float32)
    sinT = np.zeros((96, S), np.float32)
    for p in range(96):
        d = p % 32
        j = d % 16
        cosT[p] = cos[:, j]
        sinT[p] = -sin[:, j] if d < 16 else sin[:, j]
    c["c_cos"] = cosT
    c["c_sin"] = sinT
    rc = np.zeros((2, 128, 2, T), np.float32)
    wins = {(0, 0): 2, (0, 1): 4, (1, 0): 8, (1, 1): 16}
    tt = np.arange(T)
    for ch in range(2):
        for hh in range(2):
            w = wins[(ch, hh)]
            rc[0, hh * 64:(hh + 1) * 64, ch, :] = 1.0 / np.minimum(tt + 1, w)
            rc[1, hh * 64:(hh + 1) * 64, ch, :] = 1.0 / w
    c["c_rc"] = rc
    return c


WEIGHT_SPECS = [
    ("w_in", [DEPTH, D, IN_COLS]), ("pool_w", [DEPTH, 4, 64, 64]), ("pool_scale", [DEPTH, 256]),
    ("conv_w", [DEPTH, 3, 256]), ("lam_q1", [DEPTH, 32]), ("lam_k1", [DEPTH, 32]), ("lam_q2", [DEPTH, 32]),
    ("lam_k2", [DEPTH, 32]), ("subln_g", [DEPTH, 64]), ("sg_ln_g", [DEPTH, 256]), ("sg_ln_b", [DEPTH, 256]),
    ("sg_w", [DEPTH, 4, 128, 128]), ("sg_b", [DEPTH, 4, 128]), ("w_branch", [DEPTH, 4, 256, D]),
    ("w_o", [DEPTH, D, D]), ("ln1_g", [DEPTH, D]), ("ln1_b", [DEPTH, D]), ("w_rg", [DEPTH, D, 4]),
    ("b_rg", [DEPTH, 4]), ("w_re", [DEPTH, 4, D, 4]), ("b_re", [DEPTH, 4, 4]), ("w_gate", [DEPTH, 16, D, 256]),
    ("w_up", [DEPTH, 16, D, 256]), ("w_down", [DEPTH, 16, 256, D]), ("ln2_g", [DEPTH, D]), ("ln2_b", [DEPTH, D]),
]
CONST_SPECS = [("c_ident", [128, 128]), ("c_maskneg", [128, 128]), ("c_tril", [128, 128]), ("c_perm", [96, 96]),
               ("c_sel", [16, 16]), ("c_cos", [96, S]), ("c_sin", [96, S]), ("c_rc", [2, 128, 2, T])]


def build(NT=NTILES, NL=DEPTH, dbg=()):
    nc = bass.Bass("TRN2", target_bir_lowering=False)
    dr = {}
    dr["x"] = nc.dram_tensor("x", [S, D], F32, kind="ExternalInput").ap()
    for name, shape in WEIGHT_SPECS + CONST_SPECS:
        dr[name] = nc.dram_tensor(name, shape, F32, kind="ExternalInput").ap()
    out_d = nc.dram_tensor("out", [S, D], F32, kind="ExternalOutput").ap()
    xs_d = nc.dram_tensor("xscratch", [S, D], F32).ap()
    NBLK = 2 * 90
    wscr = nc.dram_tensor("wscratch", [NBLK, 128, 4096], BF16).ap()
    dbg_d = {}
    for name, shape in dbg:
        dbg_d[name] = nc.dram_tensor("dbg_" + name, shape, F32, kind="ExternalOutput").ap()

    st = ExitStack()
    with st:
        def sb(name, shape, dt):
            return st.enter_context(nc.sbuf_tensor(name, shape, dt))

        def ps(name, shape, dt):
            return st.enter_context(nc.psum_tensor(name, shape, dt))

        P = Prog(nc, same_engine_sync=int(os.environ.get("K_SAME", "2")))

        PB = [ps("pb%d" % i, [128, 512], F32) for i in range(8)]
        BK = [Buf("bank%d" % i, excl=True) for i in range(8)]

        def pbf(i):
            return PB[i][:].bitcast(BF16)

        X = sb("X", [128, 4, D], F32)
        XT = sb("XT", [128, 8, T], BF16)
        XN = sb("XN", [128, 4, D], BF16)
        KC = sb("KC", [96, 3, S], BF16)
        QRZ = sb("QRZ", [96, 8, T], BF16)
        VC = sb("VC", [128, 32, 4, 65], BF16)
        NW = int(os.environ.get('K_NW', '3'))
        WS = [sb("WS%d" % i, [128, 8, 512], BF16) for i in range(NW)]
        NWD = int(os.environ.get('K_NWD', '4'))
        WD = [sb("WD%d" % i, [128, 2, 512], BF16) for i in range(NWD)]
        R = sb("R", [128, 18432], BF16)
        NTMP = 6
        TMP = [sb("TMP%d" % i, [128, 528], F32) for i in range(NTMP)]
        NPT = 3
        PT = [sb("PT%d" % i, [128, 512], BF16) for i in range(NPT)]
        LNG = sb("LNG", [128, D], F32)
        LNB = sb("LNB", [128, D], F32)
        A_ = [sb("Apool%d" % c, [128, 528], F32) for c in range(2)]
        Z_ = [sb("Zconv%d" % c, [128, 514], F32) for c in range(2)]
        PW = sb("PW", [128, 2, 128], BF16)
        PSC = sb("PSC", [128, 2], F32)
        CW = sb("CW", [128, 2, 3], F32)
        LAMV = sb("LAMV", [128, 4, 32], F32)
        LAMT = sb("LAMT", [128, 8], F32)
        SUBG = sb("SUBG", [128, 64], F32)
        SGG = sb("SGG", [128, 256], F32)
        SGBb = sb("SGBb", [128, 256], F32)
        SGWn = sb("SGWn", [128, 4, 128], BF16)
        SGW = sb("SGW", [128, 4, 128], BF16)
        SGB = sb("SGB", [128, 4], F32)
        WR = sb("WR", [128, 8, 20], BF16)
        BR = sb("BR", [128, 20], F32)
        IDf = sb("IDf", [128, 128], F32)
        ID = sb("ID", [128, 128], BF16)
        MNf = sb("MNf", [128, 128], F32)
        MN = sb("MN", [128, 128], BF16)
        TRIL = sb("TRIL", [128, 128], F32)
        PERMf = sb("PERMf", [96, 96], F32)
        PERM = sb("PERM", [96, 96], BF16)
        SELf = sb("SELf", [16, 16], F32)
        SEL = sb("SEL", [128, 16, 128], BF16)
        GTT = sb("GTT", [128, 512], BF16)
        ZER = sb("ZER", [128, 512], BF16)
        COS = sb("COS", [96, T], F32)
        SIN = sb("SIN", [96, T], F32)
        RC = sb("RC", [128, 2, T], F32)
        SM = sb("SM", [128, 864], F32)
        DTP = sb("DTP", [128, 2, 512], BF16)

        MACC = R[:, 0:8192].bitcast(F32).rearrange("p (j t) -> p j t", j=8)
        MT = R[:, 8192:12288].rearrange("p (j t) -> p j t", j=8)
        YT = [R[:, 12288 + i * 1024:12288 + (i + 1) * 1024].rearrange("p (c t) -> p c t", c=2) for i in range(4)]
        YC = R[:, 16384:17408].rearrange("p (s c) -> p s c", s=4)
        YD = R[:, 17408:18432].rearrange("p (s c) -> p s c", s=4)

        ACTT = R[:, 0:16384].rearrange("p (c t) -> p c t", c=32)

        B = {}
        for n in ["X", "XT", "VC", "MACC", "MT", "YC", "YD", "QR", "LNG", "LNB", "A0", "A1", "Z0", "Z1",
                  "PW", "PWf", "PSC", "CW", "LAMV", "LAMT", "SUBG", "SGG", "SGBb", "SGWn", "SGW", "SGB", "WR", "BR",
                  "IDf", "ID", "MNf", "MN", "TRIL", "PERMf", "PERM", "SELf", "SEL", "ZER", "COS", "SIN", "RC", "SM",
                  "out", "ACTT", "GTT", "QRZ", "XN", "DTP0", "DTP1"]:
            B[n] = Buf(n)
        for i in range(4):
            B["YT%d" % i] = Buf("YT%d" % i)
        BKC = [Buf("KC%d" % t) for t in range(NTILES)]
        BWS = [Buf("WS%d" % i) for i in range(NW)]
        BWD = [Buf("WD%d" % i) for i in range(NWD)]
        BTMP = [Buf("TMP%d" % i) for i in range(NTMP)]
        BPT = [Buf("PT%d" % i) for i in range(NPT)]
        BXS = [Buf("XS%d" % t) for t in range(NTILES)]
        RB = [Buf("RB%d" % i) for i in range(39)]
        BACT = [RB[i] for i in range(32)]

        def bMACC(j):
            return [RB[2 * j], RB[2 * j + 1]]

        def bMT(j):
            return [RB[16 + j]]

        def bYT(i):
            return [RB[24 + 2 * i], RB[25 + 2 * i]]

        bMTall = RB[16:24]
        bYC = [RB[32], RB[33]]
        bYD = [RB[34], RB[35]]
        BDBG = {}

        ctr = {"tmp": 0, "ws": 0, "wd": 0, "pt": 0, "ev": 0, "dbg": 0}

        def tmp():
            i = ctr["tmp"] % NTMP
            ctr["tmp"] += 1
            return TMP[i], BTMP[i]

        def wslot():
            i = ctr["ws"] % NW
            ctr["ws"] += 1
            return i

        def wdslot():
            i = ctr["wd"] % NWD
            ctr["wd"] += 1
            return i

        def ptslot():
            i = ctr["pt"] % NPT
            ctr["pt"] += 1
            return PT[i], BPT[i]

        def mm(bank_i, out_ap, lhsT, rhs, start, stop, reads):
            P.op("pe", lambda e: e.matmul(out_ap, lhsT=lhsT, rhs=rhs, start=start, stop=stop), reads=reads, writes=[BK[bank_i]])

        def tr(bank_i, out_ap, in_ap, reads, ident=None):
            idn = ID[:] if ident is None else ident
            P.op("pe", lambda e: e.transpose(out=out_ap, in_=in_ap, identity=idn), reads=list(reads) + [B["ID"]], writes=[BK[bank_i]])

        def sml(ap):
            n = 1
            for d in ap.shape[1:]:
                n *= d
            return n <= 160

        def act(out_ap, in_ap, func, reads, writes, scale=None, bias=None):
            kw = {}
            if scale is not None:
                kw["scale"] = scale
            if bias is not None:
                kw["bias"] = bias
            P.op("act", lambda e: e.activation(out=out_ap, in_=in_ap, func=func, **kw), reads=reads, writes=writes, small=sml(out_ap))

        def tt(out_ap, in0, in1, op, reads, writes, eng="dve"):
            P.op(eng, lambda e: e.tensor_tensor(out=out_ap, in0=in0, in1=in1, op=op), reads=reads, writes=writes, small=sml(out_ap))

        def ts(out_ap, in0, s1, s2, op0, op1, reads, writes, eng="dve"):
            if s2 is None:
                P.op(eng, lambda e: e.tensor_scalar(out=out_ap, in0=in0, scalar1=s1, scalar2=None, op0=op0), reads=reads, writes=writes, small=sml(out_ap))
            else:
                P.op(eng, lambda e: e.tensor_scalar(out=out_ap, in0=in0, scalar1=s1, scalar2=s2, op0=op0, op1=op1), reads=reads, writes=writes, small=sml(out_ap))

        def stt(out_ap, in0, scalar, in1, op0, op1, reads, writes, eng="dve"):
            P.op(eng, lambda e: e.scalar_tensor_tensor(out=out_ap, in0=in0, scalar=scalar, in1=in1, op0=op0, op1=op1), reads=reads, writes=writes, small=sml(out_ap))

        def cp(out_ap, in_ap, reads, writes, eng="dve"):
            if eng == "act":
                act(out_ap, in_ap, AF.Copy, reads, writes)
            else:
                P.op(eng, lambda e: e.tensor_copy(out=out_ap, in_=in_ap), reads=reads, writes=writes, small=sml(out_ap))

        def ld(eng, out_ap, in_ap, slot, writes, reads=()):
            P.dma(eng, lambda e: e.dma_start(out=out_ap, in_=in_ap), slot, reads=reads, writes=writes)

        def ld_nc(eng, out_ap, in_ap, slot, writes, reads=()):
            P.dma(eng, lambda e: e.dma_start(out=out_ap, in_=in_ap, allow_slow_non_contiguous=True), slot, reads=reads, writes=writes)

        def dbg_store(name, src_ap, srcbufs):
            if name not in dbg_d:
                return
            b = BDBG.setdefault(name, Buf("dbg_" + name))
            P.dma("pool", lambda e: e.dma_start(out=dbg_d[name], in_=src_ap), "dbg_" + name, reads=list(srcbufs), writes=[b])

        def rstd(out_ap, var_ap, bufs, mul=None):
            if mul is None:
                ts(out_ap, var_ap, LN_EPS, None, ALU.add, None, bufs, bufs)
            else:
                ts(out_ap, var_ap, mul, LN_EPS, ALU.mult, ALU.add, bufs, bufs)
            act(out_ap, out_ap, AF.Ln, bufs, bufs)
            act(out_ap, out_ap, AF.Exp, bufs, bufs, scale=-0.5)

        def evac_eng():
            ctr["ev"] += 1
            return "act" if ctr["ev"] % 2 else "dve"

        ld("sp", IDf[:], dr["c_ident"], "c0", [B["IDf"]])
        cp(ID[:], IDf[:], [B["IDf"]], [B["ID"]])
        ld("sp", MNf[:], dr["c_maskneg"], "c1", [B["MNf"]])
        cp(MN[:], MNf[:], [B["MNf"]], [B["MN"]])
        ld("sp", TRIL[:], dr["c_tril"], "c2", [B["TRIL"]])
        ld("sp", PERMf[:], dr["c_perm"], "c3", [B["PERMf"]])
        cp(PERM[:], PERMf[:], [B["PERMf"]], [B["PERM"]])
        ld("sp", SELf[:], dr["c_sel"], "c4", [B["SELf"]])
        P.op("dve", lambda e: e.memset(SEL[:], 0.0), writes=[B["SEL"]])
        P.op("dve", lambda e: e.memset(GTT[:], 0.0), writes=[B["GTT"]])
        cp(SEL[0:16], SELf[:].unsqueeze(2).to_broadcast([16, 16, 128]), [B["SELf"]], [B["SEL"]])
        P.op("dve", lambda e: e.memset(ZER[:], 0.0), writes=[B["ZER"]])
        P.op("dve", lambda e: e.memset(QRZ[:], 0.0), writes=[B["QRZ"]])
        P.op("dve", lambda e: e.memset(KC[64:96, 2, :], 0.0), writes=BKC)
        P.op("dve", lambda e: e.memset(VC[:], 1.0), writes=[B["VC"]])

        def load_layer_params(l):
            tpw, bpw = tmp()
            PWf = tpw[:, 0:256].rearrange("p (c n) -> p c n", c=2)
            P.op("dve", lambda e: e.memset(PWf, 0.0), writes=[bpw])
            for g in range(4):
                c, hh = g // 2, g % 2
                ld("sp", PWf[hh * 64:(hh + 1) * 64, c, hh * 64:(hh + 1) * 64], dr["pool_w"][l, g], "pw%d" % g, [bpw])
            cp(PW[:], PWf, [bpw], [B["PW"]])
            ld_nc("sp", PSC[:], dr["pool_scale"][l].rearrange("(c p) -> p c", p=128), "psc", [B["PSC"]])
            for j in range(3):
                ld_nc("sp", CW[:, :, j], dr["conv_w"][l, j].rearrange("(c p) -> p c", p=128), "cw%d" % j, [B["CW"]])
            for i, nm in enumerate(["lam_q1", "lam_k1", "lam_q2", "lam_k2"]):
                ld("sp", LAMV[:, i, :], dr[nm][l:l + 1, :].partition_broadcast(128) if False else dr[nm][l].partition_broadcast(128), "lam%d" % i, [B["LAMV"]])
            lam_init = 0.8 - 0.6 * math.exp(-0.3 * l)
            tt(LAMT[:, 0:1].unsqueeze(2).to_broadcast([128, 1, 32]) if False else SM[:, 0:32], LAMV[:, 0, :], LAMV[:, 1, :], ALU.mult, [B["LAMV"]], [B["SM"]])
            P.op("dve", lambda e: e.tensor_reduce(out=LAMT[:, 0:1], in_=SM[:, 0:32], axis=AX.X, op=ALU.add), small=True, reads=[B["SM"]], writes=[B["LAMT"]])
            tt(SM[:, 32:64], LAMV[:, 2, :], LAMV[:, 3, :], ALU.mult, [B["LAMV"]], [B["SM"]])
            P.op("dve", lambda e: e.tensor_reduce(out=LAMT[:, 1:2], in_=SM[:, 32:64], axis=AX.X, op=ALU.add), small=True, reads=[B["SM"]], writes=[B["LAMT"]])
            act(LAMT[:, 2:4], LAMT[:, 0:2], AF.Exp, [B["LAMT"]], [B["LAMT"]])
            stt(LAMT[:, 4:5], LAMT[:, 3:4], -lam_init, LAMT[:, 2:3], ALU.add, ALU.subtract, [B["LAMT"]], [B["LAMT"]])
            ld("sp", SUBG[:], dr["subln_g"][l].partition_broadcast(128), "subg", [B["SUBG"]])
            ts(SUBG[:], SUBG[:], 1.0 - lam_init, None, ALU.mult, None, [B["SUBG"]], [B["SUBG"]])
            ld("sp", SGG[:], dr["sg_ln_g"][l].partition_broadcast(128), "sgg", [B["SGG"]])
            ld("sp", SGBb[:], dr["sg_ln_b"][l].partition_broadcast(128), "sgbb", [B["SGBb"]])
            ld("pool", SGWn[:], dr["sg_w"][l].rearrange("g t s -> t g s"), "sgwn", [B["SGWn"]])
            for g in range(4):
                tr(g % 2, pbf(g % 2)[:, 0:128], SGWn[:, g, :], [B["SGWn"]])
                tt(SGW[:, g, :], pbf(g % 2)[:, 0:128], TRIL[:], ALU.mult, [BK[g % 2], B["TRIL"]], [B["SGW"]])
            ld_nc("sp", SGB[:], dr["sg_b"][l].rearrange("g t -> t g"), "sgb", [B["SGB"]])
            ld_nc("pool", WR[:, :, 0:4], dr["w_rg"][l].rearrange("(k p) g -> p k g", p=128), "wr0", [B["WR"]])
            for g in range(4):
                ld_nc("pool", WR[:, :, 4 + 4 * g:8 + 4 * g], dr["w_re"][l, g].rearrange("(k p) e -> p k e", p=128), "wr%d" % (g + 1), [B["WR"]])
            ld("sp", BR[:, 0:4], dr["b_rg"][l].partition_broadcast(128), "br0", [B["BR"]])
            ld("sp", BR[:, 4:20], dr["b_re"][l].rearrange("g e -> (g e)").partition_broadcast(128), "br1", [B["BR"]])

        def xT_prep(s):
            tb, bb = tmp()
            xb = tb[:].bitcast(BF16)[:, 0:1024]
            cp(xb, X[:, s, :], [B["X"]], [bb], eng="act")
            return xb, bb

        def xT_sub(s, from_xn, prep=None, evac=None):
            bi = s % 2
            if from_xn:
                for c in range(8):
                    tr(bi, pbf(bi)[:, c * 128:(c + 1) * 128], XN[:, s, c * 128:(c + 1) * 128], [B["XN"]])
            else:
                xb, bb = prep if prep is not None else xT_prep(s)
                for c in range(8):
                    tr(bi, pbf(bi)[:, c * 128:(c + 1) * 128], xb[:, c * 128:(c + 1) * 128], [bb])
            cp(XT[:, :, s * 128:(s + 1) * 128], pbf(bi).rearrange("p (c t) -> p c t", c=8), [BK[bi]], [B["XT"]],
               eng=(evac if evac is not None else ("dve" if s % 2 == 0 else "act")))

        def prefetch_xn(l, t):
            src = dr["x"] if l == 0 else xs_d
            rd = [] if l == 0 else [BXS[t]]
            ld("pool", XN[:], src[t * T:(t + 1) * T, :].rearrange("(s p) d -> p s d", p=128), "xn", [B["XN"]], reads=rd)

        scr_ids = {}
        BSCR = {}
        USE_SCR = bool(int(os.environ.get("K_SCR", "1")))

        def wld(key, dst_ap, src_ap, slot_sem, slot_buf):
            if not USE_SCR:
                ld("pool", dst_ap, src_ap, slot_sem, [slot_buf])
                return
            k_, n_ = dst_ap.shape[1], dst_ap.shape[2]
            first = key not in scr_ids
            if first:
                scr_ids[key] = len(scr_ids)
                BSCR[key] = Buf("scr%d" % scr_ids[key])
            view = wscr[scr_ids[key]][:, 0:k_ * n_].rearrange("p (k n) -> p k n", k=k_)
            if first:
                ld("pool", dst_ap, src_ap, slot_sem, [slot_buf])
                if NT > 1:
                    P.dma("sp", lambda e: e.dma_start(out=view, in_=dst_ap), "st_" + slot_sem, reads=[slot_buf], writes=[BSCR[key]])
            else:
                P.dma("sp", lambda e: e.dma_start(out=dst_ap, in_=view), slot_sem, reads=[BSCR[key]], writes=[slot_buf])

        def load_w_in_block(l, c0, width):
            i = wslot()
            wld(("w_in", l, c0), WS[i][:, :, 0:width], dr["w_in"][l, :, c0:c0 + width].rearrange("(k p) n -> p k n", p=128), "ws%d" % i, BWS[i])
            return i

        pre = {}

        bankrot = {"i": 0}

        def nbank(lo=0, hi=8):
            n = hi - lo
            b = lo + bankrot["i"] % n
            bankrot["i"] += 1
            return b

        def proj_fm(wi, col, width, bank_i):
            for k in range(8):
                mm(bank_i, PB[bank_i][0:width, :], WS[wi][:, k, col:col + width], XT[:, k, :], k == 0, k == 7, [BWS[wi], B["XT"]])

        def mixer_head(l, t):
            ld("sp", COS[:], dr["c_cos"][:, t * T:(t + 1) * T], "cos", [B["COS"]])
            ld("sp", SIN[:], dr["c_sin"][:, t * T:(t + 1) * T], "sin", [B["SIN"]])
            if t <= 1:
                ld("sp", RC[:], dr["c_rc"][min(t, 1)], "rc", [B["RC"]])
            for s_ in range(4):
                xT_sub(s_, True, evac="act")
            if t + 1 < NT:
                prefetch_xn(l, t + 1)
            elif l + 1 < NL:
                prefetch_xn(l + 1, 0)

        def mixer(l, t):
            lam_init = 0.8 - 0.6 * math.exp(-0.3 * l)
            src = dr["x"] if l == 0 else xs_d
            rd = [] if l == 0 else [BXS[t]]
            if not pre.pop("head", False):
                mixer_head(l, t)

            if "w0" in pre:
                w0, w1 = pre.pop("w0"), pre.pop("w1")
            else:
                w0 = load_w_in_block(l, 0, 512)
                w1 = load_w_in_block(l, 512, 512)
            GB = []
            for cc in range(4):
                bi = nbank()
                proj_fm(w0, cc * 128, 128, bi)
                if cc < 2:
                    A = A_[cc]
                    BA = B["A%d" % cc]
                    if t == 0:
                        P.op("act", lambda e, A=A: e.memset(A[:, 0:16], 0.0) if False else e.activation(out=A[:, 0:16], in_=ZER[:, 0:16], func=AF.Copy), reads=[B["ZER"]], writes=[BA])
                    else:
                        act(A[:, 0:16], A[:, 512:528], AF.Copy, [BA], [BA])
                    act(A[:, 16:528], PB[bi][:], AF.Copy, [BK[bi]], [BA])
                else:
                    tg, bg = tmp()
                    act(tg[:, 0:512], PB[bi][:], AF.Copy, [BK[bi]], [bg])
                    GB.append((tg, bg))
            for c in range(2):
                bgc = nbank()
                proj_fm(w1, c * 128, 128, bgc)
                bh = nbank()
                proj_fm(w1, 256 + c * 128, 128, bh)
                tg, bg = tmp()
                act(tg[:, 0:512], PB[bgc][:], AF.Copy, [BK[bgc]], [bg])
                Z = Z_[c]
                BZ = B["Z%d" % c]
                if t == 0:
                    cp(Z[:, 0:2], ZER[:, 0:2], [B["ZER"]], [BZ])
                else:
                    cp(Z[:, 0:2], Z[:, 512:514], [BZ], [BZ])
                tt(Z[:, 2:514], tg[:, 0:512], PB[bh][:], ALU.mult, [bg, BK[bh]], [BZ])
                ta, ba = tmp()
                ts(ta[:, 0:512], Z[:, 0:512], CW[:, c, 0:1], None, ALU.mult, None, [BZ, B["CW"]], [ba])
                stt(ta[:, 0:512], Z[:, 1:513], CW[:, c, 1:2], ta[:, 0:512], ALU.mult, ALU.add, [BZ, B["CW"], ba], [ba])
                stt(ta[:, 0:512], Z[:, 2:514], CW[:, c, 2:3], ta[:, 0:512], ALU.mult, ALU.add, [BZ, B["CW"], ba], [ba])
                gbt, gbb = GB[c]
                tt(YT[1][:, c, :], ta[:, 0:512], gbt[:, 0:512], ALU.mult, [ba, gbb], bYT(1))
            w2 = pre.pop("w2") if "w2" in pre else load_w_in_block(l, 1024, 512)

            def qk_proj(qk, r):
                wd_ = 96 if r < 2 else 64
                col = qk * 256 + 96 * r
                bi = nbank()
                proj_fm(w2, col, wd_, bi)
                tq, bq = tmp()
                qs = tq[:].bitcast(BF16)[0:wd_, 0:512]
                act(qs, PB[bi][0:wd_, :], AF.Copy, [BK[bi]], [bq])
                return (qk, r, wd_, bi, qs, bq)

            def qk_rot(st_):
                qk, r, wd_, bi, qs, bq = st_
                b2 = nbank()
                mm(b2, PB[b2][0:wd_, :], PERM[0:wd_, 0:wd_], qs, True, True, [B["PERM"], bq])
                t1, bt1 = tmp()
                t2, bt2 = tmp()
                tt(t1[0:wd_, 0:512], PB[bi][0:wd_, :], COS[0:wd_, :], ALU.mult, [BK[bi], B["COS"]], [bt1])
                tt(t2[0:wd_, 0:512], PB[b2][0:wd_, :], SIN[0:wd_, :], ALU.mult, [BK[b2], B["SIN"]], [bt2])
                if qk == 0:
                    for pl in range(wd_ // 32):
                        tt(QRZ[32 * pl:32 * pl + 32, 3 * r + pl, :], t1[32 * pl:32 * pl + 32, 0:512], t2[32 * pl:32 * pl + 32, 0:512], ALU.add,
                           [bt1, bt2], [B["QRZ"]])
                else:
                    tt(KC[0:wd_, r, t * T:(t + 1) * T], t1[0:wd_, 0:512], t2[0:wd_, 0:512], ALU.add, [bt1, bt2], [BKC[t]])

            qkitems = [(qk, r) for qk in range(2) for r in range(3)]
            prev = None
            for (qk, r) in qkitems:
                cur = qk_proj(qk, r)
                if prev is not None:
                    qk_rot(prev)
                prev = cur
            qk_rot(prev)

            w3 = load_w_in_block(l, 1536, 512)
            w4 = load_w_in_block(l, 2048, 256)
            def sg_proj(s):
                b1 = nbank()
                for k in range(8):
                    mm(b1, PB[b1][:], XT[:, k, s * 128:(s + 1) * 128], WS[w3][:, k, :], k == 0, k == 7, [B["XT"], BWS[w3]])
                b2 = nbank()
                for k in range(8):
                    mm(b2, PB[b2][:, 0:256], XT[:, k, s * 128:(s + 1) * 128], WS[w4][:, k, 0:256], k == 0, k == 7, [B["XT"], BWS[w4]])
                act(VC[:, t * 4 + s, :, 0:64], PB[b1][:, 0:256].rearrange("p (h e) -> p h e", h=4), AF.Copy, [BK[b1]], [B["VC"]])
                tu, bu = tmp()
                act(tu[:, 0:256], PB[b1][:, 256:512], AF.Gelu_apprx_tanh, [BK[b1]], [bu])
                tv, bv = tmp()
                act(tv[:, 0:256], PB[b2][:, 0:256], AF.Gelu_apprx_tanh, [BK[b2]], [bv])
                so = (s % 2) * 16
                P.op("dve", lambda e, tv=tv, so=so: e.bn_stats(out=SM[:, 64 + so:70 + so], in_=tv[:, 0:256]), small=True, reads=[bv], writes=[B["SM"]])
                P.op("dve", lambda e, so=so: e.bn_aggr(out=SM[:, 72 + so:74 + so], in_=SM[:, 64 + so:70 + so]), small=True, reads=[B["SM"]], writes=[B["SM"]])
                rstd(SM[:, 74 + so:75 + so], SM[:, 73 + so:74 + so], [B["SM"]])
                ts(tv[:, 0:256], tv[:, 0:256], SM[:, 72 + so:73 + so], SM[:, 74 + so:75 + so], ALU.subtract, ALU.mult, [bv, B["SM"]], [bv])
                tt(tv[:, 0:256], tv[:, 0:256], SGG[:], ALU.mult, [bv, B["SGG"]], [bv])
                vn = tv[:].bitcast(BF16)[:, 512:768]
                tt(vn, tv[:, 0:256], SGBb[:], ALU.add, [bv, B["SGBb"]], [bv])
                return tu, bu, vn, bv

            def sg_spatial(s, st_):
                tu, bu, vn, bv = st_
                b3 = nbank()
                for g in range(4):
                    mm(b3, PB[b3][:, g * 64:(g + 1) * 64], SGW[:, g, :], vn[:, g * 64:(g + 1) * 64], True, True, [B["SGW"], bv])
                tt(tu[:, 256:512].rearrange("p (g c) -> p g c", g=4), PB[b3][:, 0:256].rearrange("p (g c) -> p g c", g=4),
                   SGB[:].unsqueeze(2).to_broadcast([128, 4, 64]), ALU.add, [BK[b3], B["SGB"], bu], [bu])
                tt(YD[:, s, :], tu[:, 256:512], tu[:, 0:256], ALU.mult, [bu], bYD)

            sgst = {}
            for s in range(4):
                sgst[s] = sg_proj(s)
                if s >= 1:
                    sg_spatial(s - 1, sgst.pop(s - 1))
            sg_spatial(3, sgst.pop(3))
            bi = nbank(0, 2)
            for s in range(4):
                for c in range(2):
                    tr(bi, pbf(bi)[:, c * 512 + s * 128:c * 512 + (s + 1) * 128], YD[:, s, c * 128:(c + 1) * 128], bYD)
            cp(YT[3][:].rearrange("p c t -> p (c t)"), pbf(bi), [BK[bi]], bYT(3), eng="act")

            pool_pend = []

            def emit_pool():
                for c in range(2):
                    A = A_[c]
                    BA = B["A%d" % c]
                    sc, bsc = tmp()
                    ta, ba = tmp()
                    if c == 0:
                        tt(ta[64:128, 1:528], A[64:128, 1:528], A[64:128, 0:527], ALU.add, [BA], [ba])
                        tt(sc[64:128, 0:512], ta[64:128, 16:528], ta[64:128, 14:526], ALU.add, [ba], [bsc])
                        tt(sc[0:64, 0:512], A[0:64, 16:528], A[0:64, 15:527], ALU.add, [BA], [bsc])
                    else:
                        tb_, bb_ = tmp()
                        tt(ta[:, 1:528], A[:, 1:528], A[:, 0:527], ALU.add, [BA], [ba])
                        tt(tb_[:, 3:528], ta[:, 3:528], ta[:, 1:526], ALU.add, [ba], [bb_])
                        tt(sc[0:64, 0:512], tb_[0:64, 16:528], tb_[0:64, 12:524], ALU.add, [bb_], [bsc])
                        tt(ta[64:128, 7:528], tb_[64:128, 7:528], tb_[64:128, 3:524], ALU.add, [bb_, ba], [ba])
                        tt(sc[64:128, 0:512], ta[64:128, 16:528], ta[64:128, 8:520], ALU.add, [ba], [bsc])
                    tt(sc[:, 0:512], sc[:, 0:512], RC[:, c, :], ALU.mult, [bsc, B["RC"]], [bsc])
                    dT = DTP[:, c, :]
                    bd = B["DTP%d" % c]
                    tt(dT, sc[:, 0:512], A[:, 16:528], ALU.subtract, [bsc, BA], [bd])
                    pool_pend.append((c, dT, bd))

            def emit_pool_mm():
                for (c, dT, bd) in pool_pend:
                    bi = 3
                    mm(bi, PB[bi][:], PW[:, c, :], dT, True, True, [B["PW"], bd])
                    ts(YT[0][:, c, :], PB[bi][:], PSC[:, c:c + 1], None, ALU.mult, None, [BK[bi], B["PSC"]], bYT(0))


            nkb = 4 * t + 4
            items = [(h, kb, m) for h in range(4) for kb in range(nkb) for m in range(2)]
            st_info = {}

            def issue_st(idx):
                h, kb, m = items[idx]
                diag = kb >= 4 * t
                j = kb - 4 * t if diag else 0
                qlo = j * 128
                n = T - qlo
                pair = 2 * h + m
                r = pair // 3
                sbk = idx % 3
                mm(sbk, PB[sbk][:, 0:n], KC[0:96, r, kb * 128:(kb + 1) * 128], QRZ[0:96, pair, qlo:T], True, not diag,
                   [BKC[kb // 4], B["QRZ"]])
                if diag:
                    mm(sbk, PB[sbk][:, 0:128], ID[:], MN[:], False, True, [B["ID"], B["MN"]])
                st_info[idx] = (sbk, j, n)

            def head_banks(h):
                return [4 + 2 * (h % 2), 5 + 2 * (h % 2)]

            def finish_head(h):
                ob = head_banks(h)
                o1 = PB[ob[0]][:, 0:260].rearrange("p (q e) -> p q e", q=4)
                o2 = PB[ob[1]][:, 0:260].rearrange("p (q e) -> p q e", q=4)
                so = 128 + h * 16
                P.op("dve", lambda e, o1=o1, so=so: e.reciprocal(out=SM[:, so:so + 4], in_=o1[:, :, 64]), small=True, reads=[BK[ob[0]]], writes=[B["SM"]])
                P.op("dve", lambda e, o2=o2, so=so: e.reciprocal(out=SM[:, so + 4:so + 8], in_=o2[:, :, 64]), small=True, reads=[BK[ob[1]]], writes=[B["SM"]])
                ta, ba = tmp()
                tb_, bb_ = tmp()
                av = ta[:, 0:256].rearrange("p (q e) -> p q e", q=4)
                bv_ = tb_[:, 0:256].rearrange("p (q e) -> p q e", q=4)
                tt(av, o1[:, :, 0:64], SM[:, so:so + 4].unsqueeze(2).to_broadcast([128, 4, 64]), ALU.mult, [BK[ob[0]], B["SM"]], [ba])
                tt(bv_, o2[:, :, 0:64], SM[:, so + 4:so + 8].unsqueeze(2).to_broadcast([128, 4, 64]), ALU.mult, [BK[ob[1]], B["SM"]], [bb_])
                stt(ta[:, 0:256], tb_[:, 0:256], LAMT[:, 4:5], ta[:, 0:256], ALU.mult, ALU.add, [bb_, B["LAMT"], ba], [ba])
                tt(tb_[:, 0:256], ta[:, 0:256], ta[:, 0:256], ALU.mult, [ba], [bb_])
                P.op("dve", lambda e, bv_=bv_, so=so: e.tensor_reduce(out=SM[:, so + 8:so + 12], in_=bv_, axis=AX.X, op=ALU.add), small=True, reads=[bb_], writes=[B["SM"]])
                rstd(SM[:, so + 8:so + 12], SM[:, so + 8:so + 12], [B["SM"]], mul=1.0 / 64.0)
                tt(av, av, SM[:, so + 8:so + 12].unsqueeze(2).to_broadcast([128, 4, 64]), ALU.mult, [ba, B["SM"]], [ba])
                tt(YC[:, :, h * 64:(h + 1) * 64], av, SUBG[:].unsqueeze(1).to_broadcast([128, 4, 64]), ALU.mult, [ba, B["SUBG"]], bYC)

            LOOK = 2
            for idx in range(min(LOOK, len(items))):
                issue_st(idx)
            for idx, (h, kb, m) in enumerate(items):
                if idx == 0:
                    emit_pool()
                if idx == len(items) // 2:
                    emit_pool_mm()
                ob = head_banks(h)
                if kb == 0 and m == 0:
                    for mm_ in range(2):
                        mm(ob[mm_], PB[ob[mm_]][:], ZER[:, 0:128], ZER[:], True, False, [B["ZER"]])
                if idx + LOOK < len(items):
                    issue_st(idx + LOOK)
                sbk, j, n = st_info.pop(idx)
                pt, bpt = ptslot()
                act(pt[:, 0:n], PB[sbk][:, 0:n], AF.Exp, [BK[sbk]], [bpt], scale=SCALE)
                last = (kb == nkb - 1)
                for qs in range(j, 4):
                    P.op("pe", lambda e, ob=ob, m=m, qs=qs, pt=pt, j=j, kb=kb, h=h, last=last: e.matmul(
                        PB[ob[m]][:, qs * 65:(qs + 1) * 65], lhsT=pt[:, (qs - j) * 128:(qs - j + 1) * 128],
                        rhs=VC[:, kb, h, :], start=False, stop=last, skip_group_check=True),
                        reads=[bpt, B["VC"]], writes=[BK[ob[m]]])
                if last and m == 1:
                    finish_head(h)
            bi = nbank(0, 2)
            for s in range(4):
                for c in range(2):
                    tr(bi, pbf(bi)[:, c * 512 + s * 128:c * 512 + (s + 1) * 128], YC[:, s, c * 128:(c + 1) * 128], bYC)
            cp(YT[2][:].rearrange("p c t -> p (c t)"), pbf(bi), [BK[bi]], bYT(2), eng="act")

            if l == 0 and t == 0:
                dbg_store("xt", XT[:], [B["XT"]])
                for i in range(4):
                    dbg_store("yt%d" % i, YT[i], bYT(i))
            ld("sp", X[:], src[t * T:(t + 1) * T, :].rearrange("(s p) d -> p s d", p=128), "xload", [B["X"]], reads=rd)

            border = [1, 3, 0, 2]
            for i in border:
                for hb in range(2):
                    wb = wdslot()
                    wld(("wbr", l, i, hb), WD[wb][:], dr["w_branch"][l, i, :, hb * 512:(hb + 1) * 512].rearrange("(k p) n -> p k n", p=128), "wd%d" % wb, BWD[wb])
                    wg = load_w_in_block(l, 2304 + i * 1024 + hb * 512, 512)
                    for jj in range(4):
                        j = hb * 4 + jj
                        bg_ = nbank()
                        proj_fm(wg, jj * 128, 128, bg_)
                        bb2 = nbank()
                        for k2 in range(2):
                            mm(bb2, PB[bb2][:], WD[wb][:, k2, jj * 128:(jj + 1) * 128], YT[i][:, k2, :], k2 == 0, k2 == 1, [BWD[wb]] + bYT(i))
                        tsg, bsg = tmp()
                        act(tsg[:, 0:512], PB[bg_][:], AF.Sigmoid, [BK[bg_]], [bsg])
                        if i == border[0]:
                            tt(MACC[:, j, :], tsg[:, 0:512], PB[bb2][:], ALU.mult, [bsg, BK[bb2]], bMACC(j))
                        else:
                            tt(tsg[:, 0:512], tsg[:, 0:512], PB[bb2][:], ALU.mult, [bsg, BK[bb2]], [bsg])
                            if i != border[-1]:
                                tt(MACC[:, j, :], MACC[:, j, :], tsg[:, 0:512], ALU.add, bMACC(j) + [bsg], bMACC(j))
                            else:
                                tt(MT[:, j, :], MACC[:, j, :], tsg[:, 0:512], ALU.add, bMACC(j) + [bsg], bMT(j))

            ld("sp", LNG[:], dr["ln1_g"][l].partition_broadcast(128), "lng", [B["LNG"]])
            ld("sp", LNB[:], dr["ln1_b"][l].partition_broadcast(128), "lnb", [B["LNB"]])
            wos = []
            for hf in range(2):
                wo = wslot()
                wld(("wo", l, hf), WS[wo][:], dr["w_o"][l, :, hf * 512:(hf + 1) * 512].rearrange("(k p) n -> p k n", p=128), "ws%d" % wo, BWS[wo])
                wos.append(wo)
            preps = []
            for s in range(4):
                for hf in range(2):
                    wo = wos[hf]
                    bi = nbank(2, 8)
                    for j in range(8):
                        mm(bi, PB[bi][:], MT[:, j, s * 128:(s + 1) * 128], WS[wo][:, j, :], j == 0, j == 7, bMT(j) + [BWS[wo]])
                    stt(X[:, s, hf * 512:(hf + 1) * 512], X[:, s, hf * 512:(hf + 1) * 512], ALPHA, PB[bi][:], ALU.mult, ALU.add, [B["X"], BK[bi]], [B["X"]])
                if l == 0 and t == 0 and s == 3:
                    dbg_store("mt", MT, bMTall)
                layer_norm_s(s)
                preps.append(xT_prep(s))
                if s >= 1:
                    xT_sub(s - 1, False, preps[s - 1])
            xT_sub(3, False, preps[3])
            if l == 0 and t == 0:
                dbg_store("x1", X[:], [B["X"]])

        def layer_norm_inplace():
            for s in range(4):
                layer_norm_s(s)

        def layer_norm_s(s):
            if True:
                so = 256 + s * 32
                for hf in range(2):
                    P.op("dve", lambda e, s=s, hf=hf, so=so: e.bn_stats(out=SM[:, so + 6 * hf:so + 6 * hf + 6], in_=X[:, s, hf * 512:(hf + 1) * 512]),
                         small=True, reads=[B["X"]], writes=[B["SM"]])
                P.op("dve", lambda e, so=so: e.bn_aggr(out=SM[:, so + 12:so + 14], in_=SM[:, so:so + 12]), small=True, reads=[B["SM"]], writes=[B["SM"]])
                rstd(SM[:, so + 14:so + 15], SM[:, so + 13:so + 14], [B["SM"]])
                ts(X[:, s, :], X[:, s, :], SM[:, so + 12:so + 13], SM[:, so + 14:so + 15], ALU.subtract, ALU.mult, [B["X"], B["SM"]], [B["X"]])
                lne = os.environ.get("K_LNENG", "dve")
                tt(X[:, s, :], X[:, s, :], LNG[:], ALU.mult, [B["X"], B["LNG"]], [B["X"]], eng=lne)
                tt(X[:, s, :], X[:, s, :], LNB[:], ALU.add, [B["X"], B["LNB"]], [B["X"]], eng=lne)

        def moe(l, t, last_layer):
            rb = nbank(0, 4)
            for s in range(4):
                for k in range(8):
                    mm(rb, PB[rb][:, s * 32:s * 32 + 20], XT[:, k, s * 128:(s + 1) * 128], WR[:, k, :], k == 0, k == 7, [B["XT"], B["WR"]])
            LG = SM[:, 512:592].rearrange("p (s c) -> p s c", s=4)
            tt(LG, PB[rb][:, 0:128].rearrange("p (s c) -> p s c", s=4)[:, :, 0:20], BR[:].unsqueeze(1).to_broadcast([128, 4, 20]), ALU.add,
               [BK[rb], B["BR"]], [B["SM"]])
            gl = LG[:, :, 0:4]
            el = LG[:, :, 4:20].rearrange("p s (g e) -> p s g e", g=4)
            o = 600
            GM = SM[:, o:o + 4]
            OHG = SM[:, o + 4:o + 20].rearrange("p (s g) -> p s g", s=4)
            EG = SM[:, o + 20:o + 36].rearrange("p (s g) -> p s g", s=4)
            SG_ = SM[:, o + 36:o + 40]
            PSEL = SM[:, o + 40:o + 44]
            T4 = SM[:, o + 44:o + 108].rearrange("p (s g e) -> p s g e", s=4, g=4)
            ELS = SM[:, o + 108:o + 124].rearrange("p (s e) -> p s e", s=4)
            M1 = SM[:, o + 124:o + 128]
            OH1 = SM[:, o + 128:o + 144].rearrange("p (s e) -> p s e", s=4)
            MSK = SM[:, o + 144:o + 160].rearrange("p (s e) -> p s e", s=4)
            M2_ = SM[:, o + 160:o + 164]
            OH2 = SM[:, o + 164:o + 180].rearrange("p (s e) -> p s e", s=4)
            W1 = SM[:, o + 180:o + 184]
            W2 = SM[:, o + 184:o + 188]
            GIG = SM[:, o + 188:o + 204].rearrange("p (s e) -> p s e", s=4)
            GIG2 = SM[:, o + 204:o + 220].rearrange("p (s e) -> p s e", s=4)
            GATE = SM[:, o + 220:o + 252].bitcast(BF16).rearrange("p (s g e) -> p s g e", s=4, g=4)
            sm = [B["SM"]]

            def bc3(ap4):
                return ap4.unsqueeze(2).to_broadcast([128, 4, 4])

            P.op("dve", lambda e: e.tensor_reduce(out=GM, in_=gl, axis=AX.X, op=ALU.max), small=True, reads=sm, writes=sm)
            tt(OHG, gl, bc3(GM), ALU.is_equal, sm, sm)
            tt(EG, gl, bc3(GM), ALU.subtract, sm, sm)
            act(EG, EG, AF.Exp, sm, sm)
            P.op("dve", lambda e: e.tensor_reduce(out=SG_, in_=EG, axis=AX.X, op=ALU.add), small=True, reads=sm, writes=sm)
            P.op("dve", lambda e: e.reciprocal(out=PSEL, in_=SG_), small=True, reads=sm, writes=sm)
            tt(T4, el, OHG.unsqueeze(3).to_broadcast([128, 4, 4, 4]), ALU.mult, sm, sm)
            P.op("dve", lambda e: e.tensor_reduce(out=ELS, in_=T4.rearrange("p s g e -> p s e g"), axis=AX.X, op=ALU.add), small=True, reads=sm, writes=sm)
            P.op("dve", lambda e: e.tensor_reduce(out=M1, in_=ELS, axis=AX.X, op=ALU.max), small=True, reads=sm, writes=sm)
            tt(OH1, ELS, bc3(M1), ALU.is_equal, sm, sm)
            stt(MSK, OH1, -1e30, ELS, ALU.mult, ALU.add, sm, sm)
            P.op("dve", lambda e: e.tensor_reduce(out=M2_, in_=MSK, axis=AX.X, op=ALU.max), small=True, reads=sm, writes=sm)
            tt(OH2, MSK, bc3(M2_), ALU.is_equal, sm, sm)
            tt(W1, M1, M2_, ALU.subtract, sm, sm)
            act(W1, W1, AF.Sigmoid, sm, sm)
            tt(W1, W1, PSEL, ALU.mult, sm, sm)
            tt(W2, PSEL, W1, ALU.subtract, sm, sm)
            tt(GIG, OH1, bc3(W1), ALU.mult, sm, sm)
            tt(GIG2, OH2, bc3(W2), ALU.mult, sm, sm)
            tt(GIG, GIG, GIG2, ALU.add, sm, sm)
            tt(GATE, OHG.unsqueeze(3).to_broadcast([128, 4, 4, 4]), GIG.unsqueeze(2).to_broadcast([128, 4, 4, 4]), ALU.mult, sm, sm)
            if l == 0 and t == 0:
                dbg_store("sm", SM[:], sm)
            bgt = B["GTT"]
            GT = GTT[:]

            def gate_T():
                gb_ = nbank()
                for s in range(4):
                    tr(gb_, pbf(gb_)[0:16, s * 128:(s + 1) * 128], GATE[:, s].rearrange("p g e -> p (g e)"), sm)
                cp(GTT[0:16, :], pbf(gb_)[0:16, 0:512], [BK[gb_]], [bgt])

            def m2_expert(e_, hf):
                wd = wdslot()
                wld(("wd", l, e_, hf), WD[wd][:], dr["w_down"][l, e_, :, hf * 512:(hf + 1) * 512].rearrange("(k p) n -> p k n", p=128), "wd%d" % wd, BWD[wd])
                for hc in range(2):
                    for s in range(4):
                        bi = hf * 4 + s
                        mm(bi, PB[bi][:], ACTT[:, e_ * 2 + hc, s * 128:(s + 1) * 128], WD[wd][:, hc, :], (e_ == 0 and hc == 0), (e_ == 15 and hc == 1),
                           [BACT[e_ * 2 + hc], BWD[wd]])

            LAG = 4
            pend = {}
            wcur = {}
            for i in range(32 + LAG):
                if i < 32:
                    e_, hc = i // 2, i % 2
                    if hc == 0:
                        wi = wslot()
                        wld(("wg", l, e_), WS[wi][:, :, 0:256], dr["w_gate"][l, e_].rearrange("(k p) n -> p k n", p=128), "ws%d" % wi, BWS[wi])
                        wld(("wu", l, e_), WS[wi][:, :, 256:512], dr["w_up"][l, e_].rearrange("(k p) n -> p k n", p=128), "ws%d" % wi, BWS[wi])
                        wcur["wi"] = wi
                    wi = wcur["wi"]
                    bg_ = nbank()
                    proj_fm(wi, hc * 128, 128, bg_)
                    bu_ = nbank()
                    proj_fm(wi, 256 + hc * 128, 128, bu_)
                    tsl, bsl = tmp()
                    act(tsl[:, 0:512], PB[bg_][:], AF.Silu, [BK[bg_]], [bsl])
                    tt(tsl[:, 0:512], tsl[:, 0:512], PB[bu_][:], ALU.mult, [bsl, BK[bu_]], [bsl])
                    pend[i] = (tsl, bsl)
                if i == LAG - 1 or (LAG == 0 and i == 0):
                    gate_T()
                if i >= LAG:
                    j = i - LAG
                    e_, hc = j // 2, j % 2
                    tsl, bsl = pend.pop(j)
                    gbk = nbank()
                    mm(gbk, PB[gbk][:], SEL[:, e_, :], GT, True, True, [B["SEL"], bgt])
                    tt(ACTT[:, j, :], tsl[:, 0:512], PB[gbk][:], ALU.mult, [bsl, BK[gbk]], [BACT[j]])
            ld("sp", LNG[:], dr["ln2_g"][l].partition_broadcast(128), "lng", [B["LNG"]])
            ld("sp", LNB[:], dr["ln2_b"][l].partition_broadcast(128), "lnb", [B["LNB"]])
            nxt = (l, t + 1) if t + 1 < NT else ((l + 1, 0) if l + 1 < NL else None)
            for e_ in range(16):
                m2_expert(e_, 0)
                if e_ == 3 and nxt is not None:
                    pre["w0"] = load_w_in_block(nxt[0], 0, 512)
                    pre["w1"] = load_w_in_block(nxt[0], 512, 512)
                    pre["w2"] = load_w_in_block(nxt[0], 1024, 512)
            for s in range(4):
                stt(X[:, s, 0:512], X[:, s, 0:512], ALPHA, PB[s][:], ALU.mult, ALU.add, [B["X"], BK[s]], [B["X"]])
            for e_ in range(16):
                m2_expert(e_, 1)
            if nxt is not None:
                mixer_head(nxt[0], nxt[1])
                pre["head"] = True
            for s in range(4):
                stt(X[:, s, 512:1024], X[:, s, 512:1024], ALPHA, PB[4 + s][:], ALU.mult, ALU.add, [B["X"], BK[4 + s]], [B["X"]])
            if l == 0 and t == 0:
                dbg_store("h2pre", X[:], [B["X"]])
            layer_norm_inplace()
            if last_layer:
                P.dma("pool", lambda e: e.dma_start(out=out_d[t * T:(t + 1) * T, :].rearrange("(s p) d -> p s d", p=128), in_=X[:]), "xstore",
                      reads=[B["X"]], writes=[B["out"]])
            else:
                P.dma("pool", lambda e: e.dma_start(out=xs_d[t * T:(t + 1) * T, :].rearrange("(s p) d -> p s d", p=128), in_=X[:]), "xstore",
                      reads=[B["X"]], writes=[BXS[t]])

        prefetch_xn(0, 0)
        for l in range(NL):
            load_layer_params(l)
            for t in range(NT):
                mixer(l, t)
                moe(l, t, l == NL - 1)
        P.wait_all("pool", [B["out"]] + BXS + list(BDBG.values()))
        P.emit(st)
        build.n_ops = P.n_ops
    return nc


_CONSTS = None


def kernel(**inputs):
    global _CONSTS
    if _CONSTS is None:
        _CONSTS = host_consts()
    NT = int(os.environ.get("K_NT", NTILES))
    NL = int(os.environ.get("K_NL", DEPTH))
    nc = build(NT=NT, NL=NL)
    x = np.ascontiguousarray(np.asarray(inputs["x"], dtype=np.float32))
    shared = {}
    for name, _ in WEIGHT_SPECS:
        shared[name] = np.ascontiguousarray(np.asarray(inputs[name], dtype=np.float32))
    shared.update(_CONSTS)
    in_maps = []
    for c in range(8):
        m = dict(shared)
        m["x"] = x[c]
        in_maps.append(m)
    res = run_bass_kernel_spmd(nc, in_maps, core_ids=list(range(8)))
    out = np.stack([np.asarray(res.results[c]["out"], dtype=np.float32) for c in range(8)], axis=0)
    return out
```
